# Optimizing a Trainium2 kernel written in Bass

```python
import math
import numpy as np
import jax
import jax.numpy as jnp
from jax import lax

D_MODEL = 1024
BATCH = 4
SEQ = 8192
DEPTH = 4

GRID_W = 64
CTX_LEN = 256

H_A = 4
DK_A = 128
DV_A = 128
CONV_K = 5
GDN_CHUNK = 64
HQ_B = 8
HKV_B = 2
GROUP_B = HQ_B // HKV_B
DH_B = 64
WINDOW = 128
WIN_BLOCK = 128
ROPE_BASE = 10000.0
H_C = 4
DK_C = 128
DV_C = 128
HGRN_CHUNK = 64
N_GROUPS = 4
EXPERTS_PER_GROUP = 8
N_EXPERTS = N_GROUPS * EXPERTS_PER_GROUP
TOP_K_IN_GROUP = 2
D_EXPERT = 512
MOE_BLOCK = 128

N_BRANCH = 3
DEEPNORM_ALPHA = (2 * DEPTH) ** 0.25
DEEPNORM_BETA = (8 * DEPTH) ** -0.25
LN_EPS = 1e-5
RMS_EPS = 1e-6
L2_EPS = 1e-6
MASK_VALUE = -1e30
F_FLOOR = 1e-30

A_QKV = 2 * H_A * DK_A + H_A * DV_A
IN_SIZES = (A_QKV, 2 * H_A, 2 * H_A, H_A * DV_A,
            HQ_B * DH_B, HKV_B * DH_B, HKV_B * DH_B,
            H_C * DK_C, 2 * H_C * DK_C, H_C * DV_C, H_C * DV_C,
            N_BRANCH * D_MODEL)
D_IN = sum(IN_SIZES)
IN_SPLIT_POINTS = tuple(int(v) for v in np.cumsum(IN_SIZES)[:-1])

F32 = jnp.float32

kernel_name = 'hybrid_gdn_swa_hgrn2_hmoe_diffusion'


def layer_norm(x, g, b):
    xf = x.astype(F32)
    xc = xf - jnp.mean(xf, -1, keepdims=True)
    var = jnp.mean(xc * xc, -1, keepdims=True)
    return (xc * lax.rsqrt(var + LN_EPS) * g.astype(F32) + b.astype(F32)).astype(x.dtype)


def rms_norm(x, w):
    xf = x.astype(F32)
    return xf * lax.rsqrt(jnp.mean(xf * xf, -1, keepdims=True) + RMS_EPS) * w.astype(F32)


def l2_normalize(x):
    return x * lax.rsqrt(jnp.sum(x * x, -1, keepdims=True) + L2_EPS)


def centred_depthwise_conv(x, w):
    pad = w.shape[0] // 2
    return lax.conv_general_dilated(x, w[:, None, :].astype(x.dtype), window_strides=(1,),
                                    padding=[(pad, pad)], dimension_numbers=('NWC', 'WIO', 'NWC'),
                                    feature_group_count=x.shape[-1])


def axial_rope_tables(rows):
    row = jnp.repeat(jnp.arange(rows, dtype=F32), GRID_W)
    col = jnp.broadcast_to(jnp.arange(GRID_W, dtype=F32), (rows, GRID_W)).reshape(-1)
    quarter = DH_B // 4
    inv_freq = ROPE_BASE ** (-jnp.arange(quarter, dtype=F32) / quarter)
    ang_r = row[:, None] * inv_freq
    ang_c = col[:, None] * inv_freq
    return (jnp.cos(ang_r), jnp.sin(ang_r), jnp.cos(ang_c), jnp.sin(ang_c))


def apply_axial_rope(x, tables):
    cr, sr, cc, sc = [t[None, :, None, :] for t in tables]
    r1, r2, c1, c2 = jnp.split(x.astype(F32), 4, axis=-1)
    out = jnp.concatenate([r1 * cr - r2 * sr, r2 * cr + r1 * sr,
                           c1 * cc - c2 * sc, c2 * cc + c1 * sc], -1)
    return out.astype(x.dtype)


def _to_chunks(u, size):
    bsz, nh, t = u.shape[:3]
    return jnp.moveaxis(u.reshape(bsz, nh, t // size, size, *u.shape[3:]), 2, 0)


def _masked_exp(diff, mask):
    return jnp.where(mask, jnp.exp(jnp.where(mask, diff, 0.0)), 0.0)


def gated_delta_chunked(q, k, v, beta, g, s0):
    bsz, nh, t, _ = q.shape
    dv = v.shape[-1]
    q, k, v, beta, g = [_to_chunks(u, GDN_CHUNK) for u in (q, k, v, beta, g)]
    gc = jnp.cumsum(g, axis=-1)
    pos = jnp.arange(GDN_CHUNK)
    incl = pos[:, None] >= pos[None, :]
    decay = _masked_exp(gc[..., :, None] - gc[..., None, :], incl)
    kb = k * beta[..., None]
    m = jnp.where(pos[:, None] > pos[None, :], jnp.einsum('...id,...jd->...ij', kb, k) * decay, 0.0)
    rhs = jnp.concatenate([v * beta[..., None], kb * jnp.exp(gc)[..., None]], -1)
    eye = jnp.eye(GDN_CHUNK, dtype=F32)
    uw = lax.linalg.triangular_solve(eye + m, rhs, left_side=True, lower=True, unit_diagonal=True)
    u, w = uw[..., :dv], uw[..., dv:]
    a_qk = jnp.where(incl, jnp.einsum('...id,...jd->...ij', q, k) * decay, 0.0)
    q_dec = q * jnp.exp(gc)[..., None]
    k_dec = k * jnp.exp(gc[..., -1:] - gc)[..., None]
    g_last = jnp.exp(gc[..., -1])[..., None, None]

    def step(s, xs):
        u_c, w_c, q_c, a_c, k_c, gl_c = xs
        v_new = u_c - w_c @ s
        o = q_c @ s + a_c @ v_new
        s = gl_c * s + jnp.swapaxes(k_c, -1, -2) @ v_new
        return s, o

    s_fin, o = lax.scan(step, s0, (u, w, q_dec, a_qk, k_dec, g_last))
    return jnp.moveaxis(o, 0, 2).reshape(bsz, nh, t, dv), s_fin


def gla_chunked(q, k, v, logf, s0):
    bsz, nh, t, _ = q.shape
    dv = v.shape[-1]
    q, k, v, logf = [_to_chunks(u, HGRN_CHUNK) for u in (q, k, v, logf)]
    b = jnp.cumsum(logf, axis=-2)
    pos = jnp.arange(HGRN_CHUNK)
    incl = (pos[:, None] >= pos[None, :])[:, :, None]

    def step(s, xs):
        q_c, k_c, v_c, b_c = xs
        decay = _masked_exp(b_c[..., :, None, :] - b_c[..., None, :, :], incl)
        a = jnp.einsum('bhtd,bhsd,bhtsd->bhts', q_c, k_c, decay)
        b_last = b_c[..., -1:, :]
        o = (q_c * jnp.exp(b_c)) @ s + a @ v_c
        s = jnp.exp(b_last)[..., 0, :, None] * s + jnp.swapaxes(k_c * jnp.exp(b_last - b_c), -1, -2) @ v_c
        return s, o

    s_fin, o = lax.scan(step, s0, (q, k, v, b))
    return jnp.moveaxis(o, 0, 2).reshape(bsz, nh, t, dv), s_fin


def _flip_fn(direction):
    return (lambda u: u) if direction == 0 else (lambda u: jnp.flip(u, axis=2))


def gdn_mixer(ctx_parts, lat_parts, conv_w, a_log, dt_bias, norm_w, need_ctx):
    def prep(qkv_raw, beta_logit, alpha_logit):
        bsz, t, _ = qkv_raw.shape
        qkv = jax.nn.silu(centred_depthwise_conv(qkv_raw, conv_w)).astype(F32)
        q, k, v = jnp.split(qkv, [H_A * DK_A, 2 * H_A * DK_A], -1)
        heads = lambda u, dh: u.reshape(bsz, t, H_A, dh).transpose(0, 2, 1, 3)
        q = l2_normalize(heads(q, DK_A)) * DK_A ** -0.5
        k = l2_normalize(heads(k, DK_A))
        v = heads(v, DV_A)
        per_dir = lambda u: u.astype(F32).reshape(bsz, t, 2, H_A).transpose(2, 0, 3, 1)
        beta = jax.nn.sigmoid(per_dir(beta_logit))
        g = -jnp.exp(a_log.astype(F32))[:, None, :, None] * jax.nn.softplus(
            per_dir(alpha_logit) + dt_bias.astype(F32)[:, None, :, None])
        return q, k, v, beta, g

    def finish(o, gate_raw):
        bsz, _, t, _ = o.shape
        o = jnp.transpose(rms_norm(o, norm_w), (0, 2, 1, 3)).reshape(bsz, t, H_A * DV_A)
        return (o * jax.nn.silu(gate_raw.astype(F32))).astype(gate_raw.dtype)

    qc, kc, vc, beta_c, g_c = prep(*ctx_parts[:3])
    ql, kl, vl, beta_l, g_l = prep(*lat_parts[:3])
    oc = 0.0
    ol = 0.0
    for direction in range(2):
        f = _flip_fn(direction)
        s0 = jnp.zeros((qc.shape[0], H_A, DK_A, DV_A), F32)
        o_c, s_ctx = gated_delta_chunked(f(qc), f(kc), f(vc), f(beta_c[direction]), f(g_c[direction]), s0)
        o_l, _ = gated_delta_chunked(f(ql), f(kl), f(vl), f(beta_l[direction]), f(g_l[direction]), s_ctx)
        oc = oc + f(o_c)
        ol = ol + f(o_l)
    yl = finish(ol, lat_parts[3])
    yc = finish(oc, ctx_parts[3]) if need_ctx else None
    return yc, yl


def window_attention_mixer(ctx_parts, lat_parts, rope, sink, need_ctx):
    qc_raw, kc_raw, vc_raw = ctx_parts
    ql_raw, kl_raw, vl_raw = lat_parts
    bsz, n_lat, _ = ql_raw.shape
    n_ctx = kc_raw.shape[1]
    nb = n_lat // WIN_BLOCK
    scale = DH_B ** -0.5
    ql = apply_axial_rope(ql_raw.reshape(bsz, n_lat, HQ_B, DH_B), rope)
    kl = apply_axial_rope(kl_raw.reshape(bsz, n_lat, HKV_B, DH_B), rope)
    vl = vl_raw.reshape(bsz, n_lat, HKV_B, DH_B)
    kc = kc_raw.reshape(bsz, n_ctx, HKV_B, DH_B)
    vc = vc_raw.reshape(bsz, n_ctx, HKV_B, DH_B)
    sink_f = sink.astype(F32).reshape(HKV_B, GROUP_B)

    qb = ql.reshape(bsz, nb, WIN_BLOCK, HKV_B, GROUP_B, DH_B)

    def band(u):
        up = jnp.pad(u, ((0, 0), (WIN_BLOCK, WIN_BLOCK), (0, 0), (0, 0))).reshape(bsz, nb + 2, WIN_BLOCK, HKV_B, DH_B)
        return jnp.concatenate([up[:, :-2], up[:, 1:-1], up[:, 2:]], axis=2)

    kw, vw = band(kl), band(vl)
    s_win = jnp.einsum('bnqhgd,bnkhd->bnhgqk', qb, kw).astype(F32) * scale
    q_pos = jnp.arange(n_lat).reshape(nb, WIN_BLOCK)
    k_pos = (jnp.arange(nb)[:, None] - 1) * WIN_BLOCK + jnp.arange(3 * WIN_BLOCK)[None, :]
    allowed = ((jnp.abs(q_pos[:, :, None] - k_pos[:, None, :]) <= WINDOW)
               & (k_pos[:, None, :] >= 0) & (k_pos[:, None, :] < n_lat))
    s_win = jnp.where(allowed[None, :, None, None], s_win, MASK_VALUE)
    s_ctx = jnp.einsum('bnqhgd,bchd->bnhgqc', qb, kc).astype(F32) * scale
    sink_b = sink_f[None, None, :, :, None, None]
    m = jnp.maximum(jnp.maximum(s_win.max(-1, keepdims=True), s_ctx.max(-1, keepdims=True)), sink_b)
    p_win = jnp.exp(s_win - m)
    p_ctx = jnp.exp(s_ctx - m)
    inv = 1.0 / (p_win.sum(-1, keepdims=True) + p_ctx.sum(-1, keepdims=True) + jnp.exp(sink_b - m))
    ol = (jnp.einsum('bnhgqk,bnkhd->bnqhgd', (p_win * inv).astype(vw.dtype), vw)
          + jnp.einsum('bnhgqc,bchd->bnqhgd', (p_ctx * inv).astype(vc.dtype), vc))
    yl = ol.reshape(bsz, n_lat, HQ_B * DH_B).astype(ql_raw.dtype)

    yc = None
    if need_ctx:
        qcb = qc_raw.reshape(bsz, n_ctx, HKV_B, GROUP_B, DH_B)
        s = jnp.einsum('bqhgd,bkhd->bhgqk', qcb, kc).astype(F32) * scale
        sink_c = sink_f[None, :, :, None, None]
        mc = jnp.maximum(s.max(-1, keepdims=True), sink_c)
        p = jnp.exp(s - mc)
        p = p / (p.sum(-1, keepdims=True) + jnp.exp(sink_c - mc))
        oc = jnp.einsum('bhgqk,bkhd->bqhgd', p.astype(vc.dtype), vc)
        yc = oc.reshape(bsz, n_ctx, HQ_B * DH_B).astype(qc_raw.dtype)
    return yc, yl


def hgrn_mixer(ctx_parts, lat_parts, lower_bound, norm_w, need_ctx):
    lbf = lower_bound.astype(F32)

    def prep(q_raw, f_raw, i_raw):
        bsz, t, _ = q_raw.shape
        heads = lambda u: u.reshape(bsz, t, H_C, -1).transpose(0, 2, 1, 3)
        q = heads(jax.nn.silu(q_raw.astype(F32)))
        v = heads(i_raw.astype(F32))
        x = f_raw.astype(F32).reshape(bsz, t, 2, H_C * DK_C)
        logf = jnp.log(jnp.maximum(lbf + (1.0 - lbf) * jax.nn.sigmoid(x), F_FLOOR))
        k = (1.0 - lbf) * jax.nn.sigmoid(-x)
        per_dir = lambda u: u.reshape(bsz, t, 2, H_C, DK_C).transpose(2, 0, 3, 1, 4)
        return q, per_dir(k), v, per_dir(logf)

    def finish(o, gate_raw):
        bsz, _, t, _ = o.shape
        o = jnp.transpose(rms_norm(o, norm_w), (0, 2, 1, 3)).reshape(bsz, t, H_C * DV_C)
        return (o * jax.nn.silu(gate_raw.astype(F32))).astype(gate_raw.dtype)

    qc, kc, vc, logf_c = prep(*ctx_parts[:3])
    ql, kl, vl, logf_l = prep(*lat_parts[:3])
    oc = 0.0
    ol = 0.0
    for direction in range(2):
        f = _flip_fn(direction)
        s0 = jnp.zeros((qc.shape[0], H_C, DK_C, DV_C), F32)
        o_c, s_ctx = gla_chunked(f(qc), f(kc[direction]), f(vc), f(logf_c[direction]), s0)
        o_l, _ = gla_chunked(f(ql), f(kl[direction]), f(vl), f(logf_l[direction]), s_ctx)
        oc = oc + f(o_c)
        ol = ol + f(o_l)
    yl = finish(ol, lat_parts[3])
    yc = finish(oc, ctx_parts[3]) if need_ctx else None
    return yc, yl


def branch_merge(ya, yb, yc, gate_logits, wa, wb, wc, wo):
    gate_a, gate_b, gate_c = jnp.split(jax.nn.sigmoid(gate_logits), N_BRANCH, -1)
    merged = gate_a * (ya @ wa) + gate_b * (yb @ wb) + gate_c * (yc @ wc)
    return merged @ wo


def hierarchical_moe(h, w_group, b_group, w_router, b_router, w1, w3, w2):
    n_tok, d = h.shape
    group_logits = (h @ w_group).astype(F32) + b_group.astype(F32)
    group_idx = jnp.argmax(group_logits, axis=-1).astype(jnp.int32)
    group_w = jnp.take_along_axis(jax.nn.softmax(group_logits, -1), group_idx[:, None], -1)
    exp_logits = ((h @ w_router).astype(F32) + b_router.astype(F32)).reshape(n_tok, N_GROUPS, EXPERTS_PER_GROUP)
    in_group = jnp.take_along_axis(exp_logits, group_idx[:, None, None], 1)[:, 0]
    top_logit, top_idx = lax.top_k(in_group, TOP_K_IN_GROUP)
    weights = jax.nn.softmax(top_logit, -1) * group_w
    expert_id = group_idx[:, None] * EXPERTS_PER_GROUP + top_idx.astype(jnp.int32)

    n_assign = n_tok * TOP_K_IN_GROUP
    flat_e = expert_id.reshape(-1)
    flat_tok = jnp.repeat(jnp.arange(n_tok, dtype=jnp.int32), TOP_K_IN_GROUP)
    order = jnp.argsort(flat_e)
    sorted_e = flat_e[order]
    counts = jnp.zeros((N_EXPERTS,), jnp.int32).at[flat_e].add(1)
    padded = (counts + MOE_BLOCK - 1) // MOE_BLOCK * MOE_BLOCK
    pad_end = jnp.cumsum(padded)
    pad_start = pad_end - padded
    start = jnp.cumsum(counts) - counts
    dest = pad_start[sorted_e] + jnp.arange(n_assign, dtype=jnp.int32) - start[sorted_e]
    n_blocks = -(-n_assign // MOE_BLOCK) + N_EXPERTS
    cap = n_blocks * MOE_BLOCK
    slot_tok = jnp.full((cap,), n_tok, jnp.int32).at[dest].set(flat_tok[order])
    slot_w = jnp.zeros((cap,), F32).at[dest].set(weights.reshape(-1)[order])
    block_expert = jnp.minimum(jnp.searchsorted(pad_end, jnp.arange(n_blocks, dtype=jnp.int32) * MOE_BLOCK,
                                                side='right'), N_EXPERTS - 1)
    h_pad = jnp.concatenate([h, jnp.zeros((1, d), h.dtype)], 0)

    def run_block(args):
        tok, e = args
        xb = h_pad[tok]
        hid = jax.nn.silu(xb @ w1[e]) * (xb @ w3[e])
        return hid @ w2[e]

    y = lax.map(run_block, (slot_tok.reshape(n_blocks, MOE_BLOCK), block_expert))
    y = y.reshape(cap, d).astype(F32) * slot_w[:, None]
    return jnp.zeros((n_tok + 1, d), F32).at[slot_tok].add(y)[:n_tok].astype(h.dtype)


def setup_inputs(seed: int = 0) -> dict:
    key = jax.random.key(seed)
    ks = list(jax.random.split(key, 32))

    def nrm(i, shape, scale):
        return jax.random.normal(ks[i], shape, F32) * scale

    d = D_MODEL
    dt = jnp.exp(jax.random.uniform(ks[8], (DEPTH, 2, H_A), F32, math.log(1e-3), math.log(1e-1)))
    return {
        'x': nrm(0, (BATCH, SEQ, d), 1.0),
        'c': nrm(1, (BATCH, d), 1.0),
        'ctx': nrm(2, (BATCH, CTX_LEN, d), 1.0),
        'c_ctx': nrm(3, (d,), 1.0),
        'w_mod': nrm(4, (DEPTH, d, 6 * d), d ** -0.5),
        'b_mod': nrm(5, (DEPTH, 6 * d), 0.02),
        'w_in': nrm(6, (DEPTH, d, D_IN), d ** -0.5),
        'conv_a': nrm(7, (DEPTH, CONV_K, A_QKV), CONV_K ** -0.5),
        'gdn_a_log': jnp.log(jax.random.uniform(ks[9], (DEPTH, 2, H_A), F32, 1.0, 16.0)),
        'gdn_dt_bias': dt + jnp.log(-jnp.expm1(-dt)),
        'gdn_norm_w': 1.0 + nrm(10, (DEPTH, DV_A), 0.02),
        'attn_sink': nrm(11, (DEPTH, HQ_B), 0.5),
        'hgrn_lb_logits': nrm(12, (2, DEPTH, H_C * DK_C), 0.1),
        'hgrn_norm_w': 1.0 + nrm(13, (DEPTH, DV_C), 0.02),
        'w_branch_a': nrm(14, (DEPTH, H_A * DV_A, d), (H_A * DV_A) ** -0.5 * DEEPNORM_BETA),
        'w_branch_b': nrm(15, (DEPTH, HQ_B * DH_B, d), (HQ_B * DH_B) ** -0.5 * DEEPNORM_BETA),
        'w_branch_c': nrm(16, (DEPTH, H_C * DV_C, d), (H_C * DV_C) ** -0.5 * DEEPNORM_BETA),
        'w_out': nrm(17, (DEPTH, d, d), d ** -0.5 * DEEPNORM_BETA),
        'ln1_g': 1.0 + nrm(18, (DEPTH, d), 0.02),
        'ln1_b': nrm(19, (DEPTH, d), 0.02),
        'ln2_g': 1.0 + nrm(20, (DEPTH, d), 0.02),
        'ln2_b': nrm(21, (DEPTH, d), 0.02),
        'w_group': nrm(22, (DEPTH, d, N_GROUPS), d ** -0.5),
        'b_group': nrm(23, (DEPTH, N_GROUPS), 0.01),
        'w_router': nrm(24, (DEPTH, d, N_EXPERTS), d ** -0.5),
        'b_router': nrm(25, (DEPTH, N_EXPERTS), 0.01),
        'w1': nrm(26, (DEPTH, N_EXPERTS, d, D_EXPERT), d ** -0.5),
        'w3': nrm(27, (DEPTH, N_EXPERTS, d, D_EXPERT), d ** -0.5),
        'w2': nrm(28, (DEPTH, N_EXPERTS, D_EXPERT, d), D_EXPERT ** -0.5 * DEEPNORM_BETA),
    }


def reference(x, c, ctx, c_ctx, w_mod, b_mod, w_in, conv_a, gdn_a_log, gdn_dt_bias, gdn_norm_w,
              attn_sink, hgrn_lb_logits, hgrn_norm_w, w_branch_a, w_branch_b, w_branch_c, w_out,
              ln1_g, ln1_b, ln2_g, ln2_b, w_group, b_group, w_router, b_router, w1, w3, w2):
    bsz, n_lat, d = x.shape
    rows = n_lat // GRID_W
    rope = axial_rope_tables(rows)
    lb_soft = jax.nn.softmax(hgrn_lb_logits.astype(F32), axis=1)
    lower_bounds = jnp.cumsum(lb_soft, axis=1) - lb_soft[:, :1]

    xl, xc = x, ctx
    for layer in range(DEPTH):
        need_ctx = layer < DEPTH - 1
        mod_l = jax.nn.silu(c) @ w_mod[layer] + b_mod[layer]
        mod_c = jax.nn.silu(c_ctx) @ w_mod[layer] + b_mod[layer]
        sh1_l, sc1_l, g1_l, sh2_l, sc2_l, g2_l = jnp.split(mod_l[:, None, :], 6, -1)
        sh1_c, sc1_c, g1_c, sh2_c, sc2_c, g2_c = jnp.split(mod_c, 6, -1)

        hl = xl * (1.0 + sc1_l) + sh1_l
        hc = xc * (1.0 + sc1_c) + sh1_c
        pl = jnp.split(hl @ w_in[layer], IN_SPLIT_POINTS, -1)
        pc = jnp.split(hc @ w_in[layer], IN_SPLIT_POINTS, -1)
        ya_c, ya_l = gdn_mixer(pc[0:4], pl[0:4], conv_a[layer], gdn_a_log[layer], gdn_dt_bias[layer],
                               gdn_norm_w[layer], need_ctx)
        yb_c, yb_l = window_attention_mixer(pc[4:7], pl[4:7], rope, attn_sink[layer], need_ctx)
        yc_c, yc_l = hgrn_mixer(pc[7:11], pl[7:11], lower_bounds[:, layer], hgrn_norm_w[layer], need_ctx)
        out_l = branch_merge(ya_l, yb_l, yc_l, pl[11], w_branch_a[layer], w_branch_b[layer],
                             w_branch_c[layer], w_out[layer])
        xl = layer_norm(DEEPNORM_ALPHA * xl + g1_l * out_l, ln1_g[layer], ln1_b[layer])
        if need_ctx:
            out_c = branch_merge(ya_c, yb_c, yc_c, pc[11], w_branch_a[layer], w_branch_b[layer],
                                 w_branch_c[layer], w_out[layer])
            xc = layer_norm(DEEPNORM_ALPHA * xc + g1_c * out_c, ln1_g[layer], ln1_b[layer])

        h2l = (xl * (1.0 + sc2_l) + sh2_l).reshape(-1, d)
        if need_ctx:
            h2c = (xc * (1.0 + sc2_c) + sh2_c).reshape(-1, d)
            y = hierarchical_moe(jnp.concatenate([h2l, h2c], 0), w_group[layer], b_group[layer],
                                 w_router[layer], b_router[layer], w1[layer], w3[layer], w2[layer])
            y_l = y[:h2l.shape[0]].reshape(xl.shape)
            y_c = y[h2l.shape[0]:].reshape(xc.shape)
            xc = layer_norm(DEEPNORM_ALPHA * xc + g2_c * y_c, ln2_g[layer], ln2_b[layer])
        else:
            y_l = hierarchical_moe(h2l, w_group[layer], b_group[layer], w_router[layer], b_router[layer],
                                   w1[layer], w3[layer], w2[layer]).reshape(xl.shape)
        xl = layer_norm(DEEPNORM_ALPHA * xl + g2_l * y_l, ln2_g[layer], ln2_b[layer])
    return xl
```

```python
import math
from contextlib import ExitStack

import numpy as np
import concourse.bass as bass
import concourse.mybir as mybir
from concourse.bass_utils import run_bass_kernel_spmd

F32 = mybir.dt.float32
BF16 = mybir.dt.bfloat16
U32 = mybir.dt.uint32
AF = mybir.ActivationFunctionType
ALU = mybir.AluOpType
AX = mybir.AxisListType

D = 1024
NCTX = 256
DEPTH = 4
D_IN = 8464
BIG = 30000.0


class Buf:
    __slots__ = ("w", "r", "name", "excl")

    def __init__(self, name="", excl=False):
        self.w = None
        self.r = []
        self.name = name
        self.excl = excl


class Prog:
    ENGS = ("pe", "act", "dve", "pool", "sp")

    def __init__(self, nc, es, n_dma_sems=8):
        self.nc = nc
        self.stream = {e: [] for e in self.ENGS}
        self.cnt = {e: 0 for e in self.ENGS}
        self.sem = {e: es.enter_context(nc.semaphore("s_" + e)) for e in self.ENGS}
        self.waited = {e: {} for e in self.ENGS}
        self.dsem, self.dcnt, self.drr = {}, {}, {}
        for q in ("sp", "pool", "act"):
            self.dsem[q] = [es.enter_context(nc.semaphore("d_%s%d" % (q, i))) for i in range(n_dma_sems)]
            self.dcnt[q] = [0] * n_dma_sems
            self.drr[q] = 0
        self.n_ins = 0

    def _wait(self, e, dep):
        sem, val = dep
        k = id(sem)
        if self.waited[e].get(k, 0) >= val:
            return
        self.waited[e][k] = val
        self.stream[e].append(("w", sem, val))

    def _deps(self, e, reads, writes, skip_same=False):
        for b in reads:
            if b.w is not None and not (skip_same and b.w[2] == e):
                self._wait(e, b.w[:2])
            if b.excl:
                for r in b.r:
                    if r[2] != e:
                        self._wait(e, r[:2])
        for b in writes:
            if b.w is not None and not (skip_same and b.w[2] == e):
                self._wait(e, b.w[:2])
            for r in b.r:
                if not (skip_same and r[2] == e):
                    self._wait(e, r[:2])

    def op(self, e, fn, reads=(), writes=(), skip_same=False):
        self._deps(e, reads, writes, skip_same)
        self.cnt[e] += 1
        tok = (self.sem[e], self.cnt[e], e)
        self.stream[e].append(("i", fn, self.sem[e], 1))
        for b in reads:
            b.r.append(tok)
        for b in writes:
            b.w = tok
            b.r = []
        self.n_ins += 1
        return tok

    def dma(self, q, out, in_, reads=(), writes=(), **kw):
        self._deps(q, reads, writes)
        i = self.drr[q]
        self.drr[q] = (i + 1) % len(self.dsem[q])
        sem = self.dsem[q][i]
        if self.dcnt[q][i] > 0:
            self._wait(q, (sem, self.dcnt[q][i]))
        self.dcnt[q][i] += 16
        tok = (sem, self.dcnt[q][i], "dma")
        self.stream[q].append(("i", lambda eng: eng.dma_start(out=out, in_=in_, **kw), sem, 16))
        for b in reads:
            b.r.append(tok)
        for b in writes:
            b.w = tok
            b.r = []
        self.n_ins += 1
        return tok

    def barrier(self):
        for e in self.ENGS:
            for f in self.ENGS:
                if f != e and self.cnt[f] > 0:
                    self._wait(e, (self.sem[f], self.cnt[f]))
            for q in self.dsem:
                for i, s in enumerate(self.dsem[q]):
                    if self.dcnt[q][i]:
                        self._wait(e, (s, self.dcnt[q][i]))

    def emit(self):
        nc = self.nc
        streams = self.stream
        self.stream = {e: [] for e in self.ENGS}
        with nc.Block() as block:
            def mk(e):
                def body(eng):
                    for it in streams[e]:
                        if it[0] == "w":
                            eng.wait_ge(it[1], it[2])
                        else:
                            it[1](eng).then_inc(it[2], it[3])
                return body
            block.tensor(mk("pe"))
            block.scalar(mk("act"))
            block.vector(mk("dve"))
            block.gpsimd(mk("pool"))
            block.sync(mk("sp"))


class Ring:
    def __init__(self, tiles, excl=False):
        self.t = tiles
        self.b = [Buf(excl=excl) for _ in tiles]
        self.i = -1

    def next(self):
        self.i = (self.i + 1) % len(self.t)
        return self.t[self.i], self.b[self.i]


_UID = [0]


class Ctx:
    def __init__(self, nc, es, P):
        self.nc, self.es, self.P = nc, es, P

    def sb(self, shape, dt=F32, name=None):
        _UID[0] += 1
        return self.es.enter_context(self.nc.sbuf_tensor(name or ("t%d" % _UID[0]), list(shape), dt))

    def ps(self, shape, dt=F32, name=None):
        _UID[0] += 1
        return self.es.enter_context(self.nc.psum_tensor(name or ("p%d" % _UID[0]), list(shape), dt))

    def ring(self, n, shape, dt=F32, psum=False):
        return Ring([(self.ps if psum else self.sb)(shape, dt) for _ in range(n)], excl=psum)


O_QKV, O_BETA, O_ALPHA, O_GA = 0, 1536, 1544, 1552
O_BQ, O_BK, O_BV = 2064, 2576, 2704
O_CQ, O_CF, O_CI, O_CG = 2832, 3344, 4368, 4880
O_GATES = 5392

FM_TILES = ([("gq0", 128), ("gq1", 128), ("gk0", 128), ("gk1", 128), ("gv0", 128), ("gv1", 128)]
            + [("bq%d" % i, 64) for i in range(4)] + [("bqs%d" % i, 64) for i in range(4)]
            + [("bk", 64), ("bks", 64)]
            + [("cq0", 128), ("cq1", 128), ("cf00", 128), ("cf01", 128), ("cf10", 128), ("cf11", 128)])
FM_IDX = {n: i for i, (n, w) in enumerate(FM_TILES)}
NFT = len(FM_TILES)
TM_BETA, TM_ALPHA, TM_GA, TM_BV, TM_CF, TM_CI, TM_CG = 0, 4, 8, 264, 328, 840, 1096
NTM = 1352


def _rope_swap(d):
    q = d // 16
    return d + 16 if q % 2 == 0 else d - 16


def fm_columns(hh):
    cols = np.zeros((NFT, 128), np.int64)
    r = np.arange(128)
    for h in range(2):
        gh = 2 * hh + h
        cols[FM_IDX["gq%d" % h]] = O_QKV + gh * 128 + r
        cols[FM_IDX["gk%d" % h]] = O_QKV + 512 + gh * 128 + r
        cols[FM_IDX["gv%d" % h]] = O_QKV + 1024 + gh * 128 + r
        cols[FM_IDX["cq%d" % h]] = O_CQ + gh * 128 + r
        for dr in range(2):
            cols[FM_IDX["cf%d%d" % (dr, h)]] = O_CF + dr * 512 + gh * 128 + r
    d64 = np.arange(64)
    sw = np.array([_rope_swap(int(d)) for d in d64])
    for g in range(4):
        hq = hh * 4 + g
        cols[FM_IDX["bq%d" % g], :64] = O_BQ + hq * 64 + d64
        cols[FM_IDX["bqs%d" % g], :64] = O_BQ + hq * 64 + sw
    cols[FM_IDX["bk"], :64] = O_BK + hh * 64 + d64
    cols[FM_IDX["bks"], :64] = O_BK + hh * 64 + sw
    return cols.reshape(-1)


def tm_columns(hh):
    c = []
    for dr in range(2):
        for h in range(2):
            c.append(O_BETA + dr * 4 + 2 * hh + h)
    for dr in range(2):
        for h in range(2):
            c.append(O_ALPHA + dr * 4 + 2 * hh + h)
    c += list(O_GA + 2 * hh * 128 + np.arange(256))
    c += list(O_BV + hh * 64 + np.arange(64))
    for dr in range(2):
        c += list(O_CF + dr * 512 + 2 * hh * 128 + np.arange(256))
    c += list(O_CI + 2 * hh * 128 + np.arange(256))
    c += list(O_CG + 2 * hh * 128 + np.arange(256))
    assert len(c) == NTM
    return np.array(c, np.int64)


def consts_np():
    i = np.arange(128)
    c = {}
    c["ident"] = np.eye(128, dtype=np.float32)
    c["ones"] = np.ones((128, 128), np.float32)
    c["ucum"] = (i[:, None] <= i[None, :]).astype(np.float32)
    c["lcum"] = (i[:, None] >= i[None, :]).astype(np.float32)
    blk = (i[:, None] // 16) == (i[None, :] // 16)
    c["ublk"] = (c["ucum"] * blk).astype(np.float32)
    c["lblk"] = (c["lcum"] * blk).astype(np.float32)
    c["bigU"] = np.where(i[:, None] >= i[None, :], 0.0, BIG).astype(np.float32)
    c["bigL"] = np.where(i[:, None] <= i[None, :], 0.0, BIG).astype(np.float32)
    c["strictL"] = (i[:, None] > i[None, :]).astype(np.float32)
    c["strictU"] = (i[:, None] < i[None, :]).astype(np.float32)
    two = lambda m: np.concatenate([m, m], 1).astype(np.float32)
    c["ii2"] = two(np.eye(128))
    c["bd8"] = two((i[:, None] // 8) == (i[None, :] // 8))
    for s_ in (8, 16, 32, 64):
        same = (i[:, None] // (2 * s_)) == (i[None, :] // (2 * s_))
        ll = same & ((i[:, None] % (2 * s_)) >= s_) & ((i[None, :] % (2 * s_)) < s_)
        c["ms%d" % s_] = two(ll | ll.T)
    return c


def rope_tables(nlat):
    t = np.arange(nlat)
    row = (t // 64).astype(np.float32)
    col = (t % 64).astype(np.float32)
    inv = (10000.0 ** (-np.arange(16, dtype=np.float32) / 16)).astype(np.float32)
    ar = row[None, :] * inv[:, None]
    ac = col[None, :] * inv[:, None]
    C = np.concatenate([np.cos(ar), np.cos(ar), np.cos(ac), np.cos(ac)], 0)
    S = np.concatenate([-np.sin(ar), np.sin(ar), -np.sin(ac), np.sin(ac)], 0)
    return C.astype(np.float32), S.astype(np.float32)


def build_A(nlat, debug=False, phases=("proj", "attn", "hgrn", "gdn")):
    T = NCTX + nlat
    NT = T // 128
    nc = bass.Bass("TRN2", target_bir_lowering=False)
    dk = "ExternalOutput" if debug else "Internal"
    din = lambda n, s, dt=F32: nc.dram_tensor(n, list(s), dt, kind="ExternalInput").ap()
    xin = din("xin", [T, D])
    c2 = din("c2", [128, 8, 2])
    wmod = din("wmod", [D, 2048])
    bmod = din("bmod", [128, 16])
    wfm = din("wfm", [D, NFT * 128])
    wtm = din("wtm", [D, NTM])
    cn = {k: din("k_" + k, v.shape) for k, v in consts_np().items()}
    FM = nc.dram_tensor("FM", [NFT, 128, T], F32, kind=dk).ap()
    TM = nc.dram_tensor("TM", [T, NTM], F32, kind=dk).ap()
    TM2 = nc.dram_tensor("TM2", [T, 512], F32, kind=dk).ap()
    FMn = nc.dram_tensor("FMn", [4, 128, T], F32, kind=dk).ap()
    outs = {n: nc.dram_tensor(n, [T, 256], F32, kind="ExternalOutput").ap() for n in ("ya", "yb", "yc")}
    extra = {}
    if "attn" in phases:
        extra["ropeC"] = din("ropeC", [64, nlat])
        extra["ropeS"] = din("ropeS", [64, nlat])
        extra["sink"] = din("sink", [128, 4])
    if "hgrn" in phases:
        extra["lblc"] = din("lblc", [128, 4, 4])
        extra["lblr"] = din("lblr", [128, 4, 512])
        extra["lmask"] = din("lmask", [128, 4])
        extra["cnw"] = din("cnw", [128, 128])
    if "gdn" in phases:
        extra["convw"] = din("convw", [128, 5, 6])
        extra["ga"] = din("ga", [128, 4])
        extra["gdt"] = din("gdt", [128, 4])
        extra["gnw"] = din("gnw", [128, 128])

    with ExitStack() as es0:
        P = Prog(nc, es0)
        C0 = Ctx(nc, es0, P)
        K = {}
        KB = {}
        for k, ap in cn.items():
            K[k] = C0.sb(list(ap.shape), name="sk_" + k)
            KB[k] = Buf()
            P.dma("sp", K[k][:], ap[:, :], writes=[KB[k]])
        zero = C0.sb([128, 128], name="zero")
        zb = Buf()
        P.op("pool", lambda e: e.memset(zero[:], 0.0), writes=[zb])

        if "proj" in phases:
            with ExitStack() as es:
                phase_proj(nc, Ctx(nc, es, P), P, K, KB, T, xin, c2, wmod, bmod, wfm, wtm, FM, TM)
                P.barrier()
                P.emit()
        if "attn" in phases:
            with ExitStack() as es:
                phase_attn(nc, Ctx(nc, es, P), P, K, KB, T, FM, TM, extra, outs["yb"])
                P.barrier()
                P.emit()
        if "hgrn" in phases:
            with ExitStack() as es:
                phase_hgrn(nc, Ctx(nc, es, P), P, K, KB, T, FM, TM, extra, outs["yc"], zero, zb)
                P.barrier()
                P.emit()
        if "gdn" in phases:
            with ExitStack() as es:
                phase_gdn(nc, Ctx(nc, es, P), P, K, KB, T, FM, TM, extra, outs["ya"], zero, zb, TM2, FMn)
                P.barrier()
                P.emit()
    return nc


def phase_proj(nc, C, P, K, KB, T, xin, c2, wmod, bmod, wfm, wtm, FM, TM):
    NT = T // 128
    csb = C.sb([128, 8, 2]); cb = Buf()
    P.dma("sp", csb[:], c2[:, :, :], writes=[cb])
    csl = C.sb([128, 8, 2]); cslb = Buf()
    P.op("act", lambda e: e.activation(out=csl[:], in_=csb[:], func=AF.Silu), reads=[cb], writes=[cslb])
    bm = C.sb([128, 16]); bmb = Buf()
    P.dma("sp", bm[:], bmod[:, :], writes=[bmb])
    wm = C.ring(2, [128, 2048])
    pm = C.ps([128, 512]); pmb = Buf(excl=True)
    wmv = wmod.rearrange("(kt p) c -> kt p c", p=128)
    P.op("dve", lambda e: e.memset(pm[:, 0:32], 0.0), writes=[pmb])
    for kt in range(8):
        w, wb = wm.next()
        P.dma("sp" if kt % 2 == 0 else "pool", w[:], wmv[kt], writes=[wb])
        for j in range(16):
            P.op("pe", lambda e, w=w, j=j, kt=kt: e.matmul(pm[:, 2 * j:2 * j + 2], lhsT=w[:, j * 128:(j + 1) * 128],
                                                           rhs=csl[:, kt, :], start=False, stop=(kt == 7), skip_group_check=True),
                 reads=[wb, cslb], writes=[pmb], skip_same=True)
    modT = C.sb([128, 16, 2]); modb = Buf()
    P.op("dve", lambda e: e.tensor_tensor(out=modT[:], in0=pm[:, 0:32].rearrange("p (j s) -> p j s", s=2),
                                          in1=bm[:].unsqueeze(2).to_broadcast([128, 16, 2]), op=ALU.add),
         reads=[pmb, bmb], writes=[modb])
    P.op("dve", lambda e: e.tensor_scalar_add(out=modT[:, 8:16, :], in0=modT[:, 8:16, :], scalar1=1.0),
         reads=[modb], writes=[modb])

    wf = C.sb([128, 8, NFT * 128], BF16); wfb = Buf()
    wt = C.sb([128, 8, NTM], BF16); wtb = Buf()
    wfv = wfm.rearrange("(kt p) c -> kt p c", p=128)
    wtv = wtm.rearrange("(kt p) c -> kt p c", p=128)
    for kt in range(8):
        P.dma("pool", wf[:, kt, :], wfv[kt], writes=[wfb])
        P.dma("pool", wt[:, kt, :], wtv[kt], writes=[wtb])

    xr = C.ring(3, [128, D])
    hr = C.ring(2, [128, 8, 512], BF16)
    ptr = C.ring(2, [128, 512], psum=True)
    pfr = C.ring(4, [128, 512], psum=True)
    fmo = C.ring(3, [128, 512])
    tmo = C.ring(2, [128, NTM])
    groups = [(0, 2)] + [(t0, 4) for t0 in range(2, NT, 4)]
    ev = 0
    for (t0, ntile) in groups:
        ntok = ntile * 128
        s = 1 if t0 == 0 else 0
        hT, hb = hr.next()
        for i in range(ntile):
            xt, xb = xr.next()
            P.dma("sp", xt[:], xin[(t0 + i) * 128:(t0 + i + 1) * 128, :], writes=[xb])
            for half in range(2):
                pt, ptb = ptr.next()
                for j in range(4):
                    kt = half * 4 + j
                    P.op("pe", lambda e, pt=pt, xt=xt, j=j, kt=kt: e.transpose(pt[:, j * 128:(j + 1) * 128], xt[:, kt * 128:(kt + 1) * 128], K["ident"][:]),
                         reads=[xb, KB["ident"]], writes=[ptb], skip_same=True)
                for j in range(4):
                    kt = half * 4 + j
                    eng = "dve" if (ev % 2 == 0) else "act"
                    ev += 1
                    if eng == "dve":
                        fn = lambda e, pt=pt, hT=hT, j=j, kt=kt, i=i, s=s: e.tensor_scalar(
                            out=hT[:, kt, i * 128:(i + 1) * 128], in0=pt[:, j * 128:(j + 1) * 128],
                            scalar1=modT[:, 8 + kt, s:s + 1], scalar2=modT[:, kt, s:s + 1], op0=ALU.mult, op1=ALU.add)
                    else:
                        fn = lambda e, pt=pt, hT=hT, j=j, kt=kt, i=i, s=s: e.activation(
                            out=hT[:, kt, i * 128:(i + 1) * 128], in_=pt[:, j * 128:(j + 1) * 128], func=AF.Identity,
                            scale=modT[:, 8 + kt, s:s + 1], bias=modT[:, kt, s:s + 1])
                    P.op(eng, fn, reads=[ptb, modb], writes=[hb], skip_same=True)
        for ft, (nm, wd) in enumerate(FM_TILES):
            pf, pfb = pfr.next()
            for kt in range(8):
                P.op("pe", lambda e, pf=pf, ft=ft, kt=kt, wd=wd, hT=hT, ntok=ntok: e.matmul(
                    pf[0:wd, 0:ntok], lhsT=wf[:, kt, ft * 128:ft * 128 + wd], rhs=hT[:, kt, 0:ntok], start=(kt == 0), stop=(kt == 7)),
                    reads=[wfb, hb], writes=[pfb], skip_same=True)
            of, ofb = fmo.next()
            eng = "dve" if (ev % 2 == 0) else "act"
            ev += 1
            if eng == "dve":
                fn = lambda e, of=of, pf=pf, wd=wd, ntok=ntok: e.tensor_copy(out=of[0:wd, 0:ntok], in_=pf[0:wd, 0:ntok])
            else:
                fn = lambda e, of=of, pf=pf, wd=wd, ntok=ntok: e.copy(out=of[0:wd, 0:ntok], in_=pf[0:wd, 0:ntok])
            P.op(eng, fn, reads=[pfb], writes=[ofb])
            P.dma("sp", FM[ft, 0:wd, t0 * 128:t0 * 128 + ntok], of[0:wd, 0:ntok], reads=[ofb])
        for i in range(ntile):
            ot, otb = tmo.next()
            for c0 in range(0, NTM, 512):
                w = min(512, NTM - c0)
                pf, pfb = pfr.next()
                for kt in range(8):
                    P.op("pe", lambda e, pf=pf, kt=kt, hT=hT, i=i, c0=c0, w=w: e.matmul(
                        pf[:, 0:w], lhsT=hT[:, kt, i * 128:(i + 1) * 128], rhs=wt[:, kt, c0:c0 + w], start=(kt == 0), stop=(kt == 7)),
                        reads=[wtb, hb], writes=[pfb], skip_same=True)
                eng = "dve" if (ev % 2 == 0) else "act"
                ev += 1
                if eng == "dve":
                    fn = lambda e, ot=ot, pf=pf, c0=c0, w=w: e.tensor_copy(out=ot[:, c0:c0 + w], in_=pf[:, 0:w])
                else:
                    fn = lambda e, ot=ot, pf=pf, c0=c0, w=w: e.copy(out=ot[:, c0:c0 + w], in_=pf[:, 0:w])
                P.op(eng, fn, reads=[pfb], writes=[otb], skip_same=True)
            P.dma("pool", TM[(t0 + i) * 128:(t0 + i + 1) * 128, :], ot[:], reads=[otb])


def phase_attn(nc, C, P, K, KB, T, FM, TM, X, yout):
    NT = T // 128
    nlat = T - NCTX
    QrT = C.sb([64, 4, T], BF16); qrb = Buf()
    KrT = C.sb([64, T + 128], BF16); krb = Buf()
    V = C.sb([128, NT, 64], BF16); vb = Buf()
    identb = C.sb([128, 128], BF16); idb = Buf()
    P.op("dve", lambda e: e.tensor_copy(out=identb[:], in_=K["ident"][:]), reads=[KB["ident"]], writes=[idb])
    sink = C.sb([128, 4]); skb = Buf()
    P.dma("sp", sink[:], X["sink"][:, :], writes=[skb])
    masks = [C.sb([128, 384]) for _ in range(3)]
    mb = [Buf() for _ in range(3)]
    for v in range(3):
        m = masks[v]
        if v == 1:
            P.op("pool", lambda e, m=m: e.memset(m[:, 0:128], -BIG), writes=[mb[v]])
        else:
            P.op("dve", lambda e, m=m: e.tensor_scalar(out=m[:, 0:128], in0=K["bigL"][:], scalar1=-1.0, scalar2=None, op0=ALU.mult),
                 reads=[KB["bigL"]], writes=[mb[v]])
        P.op("pool", lambda e, m=m: e.memset(m[:, 128:256], 0.0), writes=[mb[v]])
        if v == 2:
            P.op("pool", lambda e, m=m: e.memset(m[:, 256:384], -BIG), writes=[mb[v]])
        else:
            P.op("dve", lambda e, m=m: e.tensor_scalar(out=m[:, 256:384], in0=K["bigU"][:], scalar1=-1.0, scalar2=None, op0=ALU.mult),
                 reads=[KB["bigU"]], writes=[mb[v]])
    P.op("pool", lambda e: e.memset(KrT[:, T:T + 128], 0.0), writes=[krb])
    tmv = TM.rearrange("(nt p) c -> p nt c", p=128)
    for a in range(0, NT, 8):
        b = min(NT, a + 8)
        P.dma("pool", V[:, a:b, :], tmv[:, a:b, TM_BV:TM_BV + 64], writes=[vb])
    ld = C.ring(4, [64, 512])
    ldc = C.ring(2, [64, 512])
    lds = C.ring(2, [64, 512])
    tmp = C.ring(3, [64, 512])
    names = ["bq0", "bq1", "bq2", "bq3", "bk"]
    snames = ["bqs0", "bqs1", "bqs2", "bqs3", "bks"]

    def dst(hi, c0, w):
        return QrT[:, hi, c0:c0 + w] if hi < 4 else KrT[:, c0:c0 + w]

    for hi in range(5):
        a, ab = ld.next()
        P.dma("sp", a[:, 0:256], FM[FM_IDX[names[hi]], 0:64, 0:256], writes=[ab])
        P.op("act", lambda e, a=a, hi=hi: e.copy(out=dst(hi, 0, 256), in_=a[:, 0:256]), reads=[ab], writes=[qrb if hi < 4 else krb])
    for c0 in range(NCTX, T, 512):
        w = min(512, T - c0)
        cc, ccb = ldc.next()
        ss, ssb = lds.next()
        P.dma("sp", cc[:, 0:w], X["ropeC"][:, c0 - NCTX:c0 - NCTX + w], writes=[ccb])
        P.dma("sp", ss[:, 0:w], X["ropeS"][:, c0 - NCTX:c0 - NCTX + w], writes=[ssb])
        for hi in range(5):
            a, ab = ld.next()
            b2, bb = ld.next()
            P.dma("sp", a[:, 0:w], FM[FM_IDX[names[hi]], 0:64, c0:c0 + w], writes=[ab])
            P.dma("pool", b2[:, 0:w], FM[FM_IDX[snames[hi]], 0:64, c0:c0 + w], writes=[bb])
            t1, t1b = tmp.next()
            P.op("dve", lambda e, t1=t1, a=a, cc=cc, w=w: e.tensor_tensor(out=t1[:, 0:w], in0=a[:, 0:w], in1=cc[:, 0:w], op=ALU.mult),
                 reads=[ab, ccb], writes=[t1b])
            P.op("pool", lambda e, b2=b2, ss=ss, w=w: e.tensor_tensor(out=b2[:, 0:w], in0=b2[:, 0:w], in1=ss[:, 0:w], op=ALU.mult),
                 reads=[bb, ssb], writes=[bb])
            P.op("dve", lambda e, t1=t1, b2=b2, hi=hi, c0=c0, w=w: e.tensor_tensor(out=dst(hi, c0, w), in0=t1[:, 0:w], in1=b2[:, 0:w], op=ALU.add),
                 reads=[t1b, bb], writes=[qrb if hi < 4 else krb])

    psw = C.ring(2, [128, 512], psum=True)
    psc = C.ring(2, [128, 512], psum=True)
    pst = C.ring(2, [128, 1024], BF16, psum=True)
    pso = C.ring(2, [128, 512], psum=True)
    swr = C.ring(2, [128, 640])
    pmr = C.ring(2, [128, 640], BF16)
    ptr = C.ring(2, [128, 640], BF16)
    smr = C.ring(4, [128, 8])
    yr = C.ring(2, [128, 256])
    for tt in range(NT):
        lat = tt >= 2
        y, yb_ = yr.next()
        if lat:
            mv = 1 if tt == 2 else (2 if tt == NT - 1 else 0)
            blocks = []
            if tt > 2:
                blocks.append((0, tt - 1))
            blocks.append((128, tt))
            if tt < NT - 1:
                blocks.append((256, tt + 1))
            blocks += [(384, 0), (512, 1)]
            ncol = 640
        else:
            blocks = [(0, 0), (128, 1)]
            ncol = 256
        for g in range(4):
            lhsT = QrT[:, g, tt * 128:(tt + 1) * 128]
            sw, swb = swr.next()
            pc, pcb = psc.next()
            if lat:
                pw, pwb = psw.next()
                P.op("pe", lambda e, pw=pw, lhsT=lhsT, tt=tt: e.matmul(pw[:, 0:384], lhsT=lhsT, rhs=KrT[:, (tt - 1) * 128:(tt + 2) * 128], start=True, stop=True),
                     reads=[qrb, krb], writes=[pwb])
                P.op("pe", lambda e, pc=pc, lhsT=lhsT: e.matmul(pc[:, 0:256], lhsT=lhsT, rhs=KrT[:, 0:256], start=True, stop=True),
                     reads=[qrb, krb], writes=[pcb])
                P.op("dve", lambda e, sw=sw, pw=pw, mv=mv: e.scalar_tensor_tensor(out=sw[:, 0:384], in0=pw[:, 0:384], scalar=0.125, in1=masks[mv][:],
                                                                                 op0=ALU.mult, op1=ALU.add), reads=[pwb, mb[mv]], writes=[swb])
                P.op("act", lambda e, sw=sw, pc=pc: e.activation(out=sw[:, 384:640], in_=pc[:, 0:256], func=AF.Copy, scale=0.125),
                     reads=[pcb], writes=[swb])
            else:
                P.op("pe", lambda e, pc=pc, lhsT=lhsT: e.matmul(pc[:, 0:256], lhsT=lhsT, rhs=KrT[:, 0:256], start=True, stop=True),
                     reads=[qrb, krb], writes=[pcb])
                P.op("act", lambda e, sw=sw, pc=pc: e.activation(out=sw[:, 0:256], in_=pc[:, 0:256], func=AF.Copy, scale=0.125),
                     reads=[pcb], writes=[swb])
            sm, smb = smr.next()
            P.op("dve", lambda e, sm=sm, sw=sw, ncol=ncol: e.reduce_max(out=sm[:, 0:1], in_=sw[:, 0:ncol], axis=AX.X), reads=[swb], writes=[smb])
            P.op("dve", lambda e, sm=sm, g=g: e.tensor_scalar(out=sm[:, 1:2], in0=sm[:, 0:1], scalar1=sink[:, g:g + 1], scalar2=-1.0, op0=ALU.max, op1=ALU.mult),
                 reads=[smb, skb], writes=[smb])
            pm, pmb = pmr.next()
            P.op("act", lambda e, pm=pm, sw=sw, sm=sm, ncol=ncol: e.activation(out=pm[:, 0:ncol], in_=sw[:, 0:ncol], func=AF.Exp, bias=sm[:, 1:2], accum_out=sm[:, 2:3]),
                 reads=[swb, smb], writes=[pmb, smb])
            P.op("act", lambda e, sm=sm, g=g: e.activation(out=sm[:, 3:4], in_=sink[:, g:g + 1], func=AF.Exp, bias=sm[:, 1:2]),
                 reads=[skb, smb], writes=[smb])
            P.op("dve", lambda e, sm=sm: e.tensor_tensor(out=sm[:, 4:5], in0=sm[:, 2:3], in1=sm[:, 3:4], op=ALU.add), reads=[smb], writes=[smb])
            P.op("dve", lambda e, sm=sm: e.reciprocal(out=sm[:, 5:6], in_=sm[:, 4:5]), reads=[smb], writes=[smb])
            pt, ptb = pst.next()
            for (co, kt_) in blocks:
                P.op("pe", lambda e, pt=pt, pm=pm, co=co: e.transpose(pt[:, co:co + 128], pm[:, co:co + 128], identb[:]),
                     reads=[pmb, idb], writes=[ptb], skip_same=True)
            PT, PTb = ptr.next()
            c_lo, c_hi = blocks[0][0], blocks[-1][0] + 128
            P.op("dve", lambda e, PT=PT, pt=pt, c_lo=c_lo, c_hi=c_hi: e.tensor_copy(out=PT[:, c_lo:c_hi], in_=pt[:, c_lo:c_hi]), reads=[ptb], writes=[PTb])
            po, pob = pso.next()
            for bi, (co, kt_) in enumerate(blocks):
                P.op("pe", lambda e, po=po, PT=PT, co=co, kt_=kt_, bi=bi, nb=len(blocks): e.matmul(po[:, 0:64], lhsT=PT[:, co:co + 128], rhs=V[:, kt_, :],
                                                                                                 start=(bi == 0), stop=(bi == nb - 1)),
                     reads=[PTb, vb], writes=[pob], skip_same=True)
            P.op("act", lambda e, y=y, po=po, sm=sm, g=g: e.activation(out=y[:, g * 64:(g + 1) * 64], in_=po[:, 0:64], func=AF.Copy, scale=sm[:, 5:6]),
                 reads=[pob, smb], writes=[yb_], skip_same=True)
        P.dma("sp", yout[tt * 128:(tt + 1) * 128, :], y[:], reads=[yb_])


import os
_CUT = int(os.environ.get('HG_CUT', '0'))


def _chunk_orders(NT):
    fwd = list(range(NT))
    bwd = [1, 0] + list(range(NT - 1, 1, -1))
    return fwd, bwd


def _finish_gated(C, P, T, Oacc, ob, TM, gcol, nw, nwb, yout, eps=1e-6):
    NT = T // 128
    gr = C.ring(2, [128, 256])
    t1r = C.ring(2, [128, 256])
    t2r = C.ring(2, [128, 256])
    str_ = C.ring(2, [128, 4])
    yr = C.ring(2, [128, 256])
    for c in range(NT):
        o = Oacc[:, c, :]
        g, gb = gr.next()
        P.dma("sp", g[:], TM[c * 128:(c + 1) * 128, gcol:gcol + 256], writes=[gb])
        t1, t1b = t1r.next()
        P.op("pool", lambda e, t1=t1, o=o: e.tensor_tensor(out=t1[:], in0=o, in1=o, op=ALU.mult), reads=[ob[c]], writes=[t1b])
        st, stb = str_.next()
        P.op("dve", lambda e, st=st, t1=t1: e.reduce_sum(out=st[:, 0:2], in_=t1[:].rearrange("p (h d) -> p h d", h=2), axis=AX.X), reads=[t1b], writes=[stb])
        P.op("dve", lambda e, st=st: e.tensor_scalar(out=st[:, 2:4], in0=st[:, 0:2], scalar1=1.0 / 128, scalar2=eps, op0=ALU.mult, op1=ALU.add), reads=[stb], writes=[stb])
        P.op("act", lambda e, st=st: e.activation(out=st[:, 2:4], in_=st[:, 2:4], func=AF.Ln), reads=[stb], writes=[stb])
        P.op("act", lambda e, st=st: e.activation(out=st[:, 0:2], in_=st[:, 2:4], func=AF.Exp, scale=-0.5), reads=[stb], writes=[stb])
        y, yb_ = yr.next()
        for h in range(2):
            P.op("dve", lambda e, y=y, o=o, st=st, h=h: e.scalar_tensor_tensor(out=y[:, h * 128:(h + 1) * 128], in0=o[:, h * 128:(h + 1) * 128], scalar=st[:, h:h + 1],
                                                                              in1=nw[:], op0=ALU.mult, op1=ALU.mult), reads=[ob[c], stb, nwb], writes=[yb_], skip_same=True)
        t2, t2b = t2r.next()
        P.op("act", lambda e, t2=t2, g=g: e.activation(out=t2[:], in_=g[:], func=AF.Exp, scale=-1.0), reads=[gb], writes=[t2b])
        P.op("dve", lambda e, t2=t2: e.tensor_scalar_add(out=t2[:], in0=t2[:], scalar1=1.0), reads=[t2b], writes=[t2b])
        P.op("dve", lambda e, t2=t2: e.reciprocal(out=t2[:], in_=t2[:]), reads=[t2b], writes=[t2b])
        P.op("pool", lambda e, t2=t2, g=g: e.tensor_tensor(out=t2[:], in0=t2[:], in1=g[:], op=ALU.mult), reads=[t2b, gb], writes=[t2b])
        P.op("dve", lambda e, y=y, t2=t2: e.tensor_tensor(out=y[:], in0=y[:], in1=t2[:], op=ALU.mult), reads=[yb_, t2b], writes=[yb_])
        P.dma("sp", yout[c * 128:(c + 1) * 128, :], y[:], reads=[yb_])


def phase_hgrn(nc, C, P, K, KB, T, FM, TM, X, yout, zero, zb):
    NT = T // 128
    Oacc = C.sb([128, NT, 256]); ob = [Buf() for _ in range(NT)]
    for c in range(NT):
        P.op("pool", lambda e, c=c: e.memset(Oacc[:, c, :], 0.0), writes=[ob[c]])
    nw = C.sb([128, 128]); nwb = Buf()
    P.dma("sp", nw[:], X["cnw"][:, :], writes=[nwb])
    lmask = C.sb([128, 4]); lmb = Buf()
    P.dma("sp", lmask[:], X["lmask"][:, :], writes=[lmb])

    def lower_bound(src, n):
        L = C.sb([128, 4, n]); Lb = Buf()
        P.dma("sp", L[:], src[:, :, :], writes=[Lb])
        mx = C.sb([128, n]); mxb = Buf()
        P.op("dve", lambda e: e.tensor_tensor(out=mx[:], in0=L[:, 0, :], in1=L[:, 1, :], op=ALU.max), reads=[Lb], writes=[mxb])
        for l in (2, 3):
            P.op("dve", lambda e, l=l: e.tensor_tensor(out=mx[:], in0=mx[:], in1=L[:, l, :], op=ALU.max), reads=[Lb, mxb], writes=[mxb])
        P.op("dve", lambda e: e.tensor_tensor(out=L[:], in0=L[:], in1=mx[:].unsqueeze(1).to_broadcast([128, 4, n]), op=ALU.subtract), reads=[Lb, mxb], writes=[Lb])
        P.op("act", lambda e: e.activation(out=L[:], in_=L[:], func=AF.Exp), reads=[Lb], writes=[Lb])
        den = C.sb([128, n]); denb = Buf()
        P.op("dve", lambda e: e.tensor_tensor(out=den[:], in0=L[:, 0, :], in1=L[:, 1, :], op=ALU.add), reads=[Lb], writes=[denb])
        for l in (2, 3):
            P.op("dve", lambda e, l=l: e.tensor_tensor(out=den[:], in0=den[:], in1=L[:, l, :], op=ALU.add), reads=[Lb, denb], writes=[denb])
        P.op("dve", lambda e: e.reciprocal(out=den[:], in_=den[:]), reads=[denb], writes=[denb])
        num = C.sb([128, n]); numb = Buf()
        P.op("dve", lambda e: e.tensor_scalar(out=num[:], in0=L[:, 0, :], scalar1=lmask[:, 0:1], scalar2=None, op0=ALU.mult), reads=[Lb, lmb], writes=[numb])
        for l in (1, 2, 3):
            P.op("dve", lambda e, l=l: e.scalar_tensor_tensor(out=num[:], in0=L[:, l, :], scalar=lmask[:, l:l + 1], in1=num[:], op0=ALU.mult, op1=ALU.add), reads=[Lb, lmb, numb], writes=[numb])
        P.op("dve", lambda e: e.tensor_tensor(out=num[:], in0=num[:], in1=den[:], op=ALU.mult), reads=[numb, denb], writes=[numb])
        return num, numb

    lbcol, lcb = lower_bound(X["lblc"], 4)
    lbrow, lrb = lower_bound(X["lblr"], 512)
    omc = C.sb([128, 4]); omr = C.sb([128, 512]); omcb, omrb = Buf(), Buf()
    P.op("dve", lambda e: e.tensor_scalar(out=omc[:], in0=lbcol[:], scalar1=-1.0, scalar2=1.0, op0=ALU.mult, op1=ALU.add), reads=[lcb], writes=[omcb])
    P.op("dve", lambda e: e.tensor_scalar(out=omr[:], in0=lbrow[:], scalar1=-1.0, scalar2=1.0, op0=ALU.mult, op1=ALU.add), reads=[lrb], writes=[omrb])
    S = {}
    Sb = {}
    KH = {}
    KHb = {}
    for dr in range(2):
        for h in range(2):
            S[dr, h] = C.sb([128, 128]); Sb[dr, h] = Buf()
            P.op("pool", lambda e, s=S[dr, h]: e.memset(s[:], 0.0), writes=[Sb[dr, h]])
            for I in range(8):
                KH[dr, h, I] = C.sb([128, 128]); KHb[dr, h, I] = Buf()
                P.op("pool", lambda e, s=KH[dr, h, I]: e.memset(s[:], 0.0), writes=[KHb[dr, h, I]])
    ldq = C.ring(4, [128, 128]); ldf = C.ring(4, [128, 128]); ldt = C.ring(4, [128, 128]); ldv = C.ring(4, [128, 128])
    qsr = C.ring(4, [128, 128]); kTr = C.ring(4, [128, 128]); lfr = C.ring(4, [128, 128]); e1r = C.ring(4, [128, 128])
    bTr = C.ring(4, [128, 128]); Qtr = C.ring(4, [128, 128]); ATr = C.ring(4, [128, 128]); qer = C.ring(4, [128, 128])
    kdr = C.ring(4, [128, 128]); Kdr = C.ring(4, [128, 128]); e2r = C.ring(4, [128, 128]); smr = C.ring(4, [128, 4])
    ps1 = C.ring(2, [128, 512], psum=True); psA = C.ring(2, [128, 512], psum=True); psK = C.ring(1, [128, 512], psum=True)
    psO = C.ring(2, [128, 512], psum=True); psS = C.ring(1, [128, 512], psum=True)
    fwd, bwd = _chunk_orders(NT)
    for step in range(NT):
        for dr in range(2):
            c = (fwd if dr == 0 else bwd)[step]
            cum, blk, msk = ("ucum", "ublk", "ucum") if dr == 0 else ("lcum", "lblk", "lcum")
            for h in range(2):
                cs = slice(c * 128, (c + 1) * 128)
                q, qb = ldq.next(); f, fb = ldf.next(); ft, ftb = ldt.next(); vi, vib = ldv.next()
                P.dma("sp", q[:], FM[FM_IDX["cq%d" % h], :, cs], writes=[qb])
                P.dma("sp", f[:], FM[FM_IDX["cf%d%d" % (dr, h)], :, cs], writes=[fb])
                P.dma("sp", ft[:], TM[cs, TM_CF + dr * 256 + h * 128:TM_CF + dr * 256 + (h + 1) * 128], writes=[ftb])
                P.dma("sp", vi[:], TM[cs, TM_CI + h * 128:TM_CI + (h + 1) * 128], writes=[vib])
                e1, e1b = e1r.next()
                P.op("act", lambda e, e1=e1, q=q: e.activation(out=e1[:], in_=q[:], func=AF.Exp, scale=-1.0), reads=[qb], writes=[e1b])
                P.op("dve", lambda e, e1=e1: e.tensor_scalar_add(out=e1[:], in0=e1[:], scalar1=1.0), reads=[e1b], writes=[e1b])
                P.op("dve", lambda e, e1=e1: e.reciprocal(out=e1[:], in_=e1[:]), reads=[e1b], writes=[e1b])
                qs, qsb = qsr.next()
                P.op("pool", lambda e, qs=qs, e1=e1, q=q: e.tensor_tensor(out=qs[:], in0=e1[:], in1=q[:], op=ALU.mult), reads=[e1b, qb], writes=[qsb])
                kT, kTb = kTr.next()
                P.op("act", lambda e, kT=kT, f=f: e.activation(out=kT[:], in_=f[:], func=AF.Exp), reads=[fb], writes=[kTb])
                P.op("dve", lambda e, kT=kT: e.tensor_scalar_add(out=kT[:], in0=kT[:], scalar1=1.0), reads=[kTb], writes=[kTb])
                P.op("dve", lambda e, kT=kT: e.reciprocal(out=kT[:], in_=kT[:]), reads=[kTb], writes=[kTb])
                P.op("dve", lambda e, kT=kT, dr=dr, h=h: e.tensor_scalar(out=kT[:], in0=kT[:], scalar1=omc[:, dr * 2 + h:dr * 2 + h + 1], scalar2=None, op0=ALU.mult),
                     reads=[kTb, omcb], writes=[kTb])
                lf, lfb = lfr.next()
                rs = slice(dr * 256 + h * 128, dr * 256 + (h + 1) * 128)
                P.op("act", lambda e, lf=lf, ft=ft: e.activation(out=lf[:], in_=ft[:], func=AF.Exp, scale=-1.0), reads=[ftb], writes=[lfb])
                P.op("dve", lambda e, lf=lf: e.tensor_scalar_add(out=lf[:], in0=lf[:], scalar1=1.0), reads=[lfb], writes=[lfb])
                P.op("dve", lambda e, lf=lf: e.reciprocal(out=lf[:], in_=lf[:]), reads=[lfb], writes=[lfb])
                P.op("pool", lambda e, lf=lf, rs=rs: e.tensor_tensor(out=lf[:], in0=lf[:], in1=omr[:, rs], op=ALU.mult), reads=[lfb, omrb], writes=[lfb])
                P.op("dve", lambda e, lf=lf, rs=rs: e.scalar_tensor_tensor(out=lf[:], in0=lf[:], scalar=1e-30, in1=lbrow[:, rs], op0=ALU.max, op1=ALU.add),
                     reads=[lfb, lrb], writes=[lfb])
                P.op("act", lambda e, lf=lf: e.activation(out=lf[:], in_=lf[:], func=AF.Ln), reads=[lfb], writes=[lfb])
                if _CUT == 1:
                    continue
                p1, p1b = ps1.next()
                P.op("pe", lambda e, p1=p1, lf=lf, cum=cum: e.matmul(p1[:, 0:128], lhsT=lf[:], rhs=K[cum][:], start=True, stop=True), reads=[lfb, KB[cum]], writes=[p1b])
                P.op("pe", lambda e, p1=p1, lf=lf, blk=blk: e.matmul(p1[:, 128:256], lhsT=lf[:], rhs=K[blk][:], start=True, stop=True), reads=[lfb, KB[blk]], writes=[p1b], skip_same=True)
                bT, bTb = bTr.next()
                P.op("dve", lambda e, bT=bT, p1=p1: e.tensor_copy(out=bT[:], in_=p1[:, 0:128]), reads=[p1b], writes=[bTb])
                e2, e2b = e2r.next()
                P.op("act", lambda e, e2=e2, p1=p1: e.activation(out=e2[:], in_=p1[:, 128:256], func=AF.Exp), reads=[p1b], writes=[e2b])
                Qt, Qtb = Qtr.next()
                P.op("pool", lambda e, Qt=Qt, e2=e2, qs=qs: e.tensor_tensor(out=Qt[:], in0=e2[:], in1=qs[:], op=ALU.mult), reads=[e2b, qsb], writes=[Qtb])
                if _CUT == 2:
                    continue
                pA, pAb = psA.next()
                for I in range(8):
                    if dr == 0:
                        lo, hi = 0, 16 * (I + 1)
                        ref = bT[:, 16 * I - 1:16 * I] if I >= 1 else 0.0
                    else:
                        lo, hi = 16 * I, 128
                        ref = bT[:, 16 * (I + 1):16 * (I + 1) + 1] if I <= 6 else 0.0
                    kh, khb = KH[dr, h, I], KHb[dr, h, I]
                    P.op("dve", lambda e, kh=kh, bT=bT, lo=lo, hi=hi, ref=ref: e.tensor_scalar(out=kh[:, lo:hi], in0=bT[:, lo:hi], scalar1=ref, scalar2=-80.0, op0=ALU.subtract, op1=ALU.max),
                         reads=[bTb], writes=[khb])
                    P.op("act", lambda e, kh=kh, lo=lo, hi=hi: e.activation(out=kh[:, lo:hi], in_=kh[:, lo:hi], func=AF.Exp, scale=-1.0),
                         reads=[khb], writes=[khb])
                    P.op("pool", lambda e, kh=kh, kT=kT, lo=lo, hi=hi: e.tensor_tensor(out=kh[:, lo:hi], in0=kh[:, lo:hi], in1=kT[:, lo:hi], op=ALU.mult),
                         reads=[kTb, khb], writes=[khb])
                    P.op("pe", lambda e, pA=pA, kh=kh, Qt=Qt, I=I: e.matmul(pA[:, 16 * I:16 * I + 16], lhsT=kh[:], rhs=Qt[:, 16 * I:16 * I + 16], start=True, stop=True),
                         reads=[khb, Qtb], writes=[pAb], skip_same=True)
                if _CUT == 3:
                    continue
                AT, ATb = ATr.next()
                P.op("dve", lambda e, AT=AT, pA=pA, msk=msk: e.tensor_tensor(out=AT[:], in0=pA[:, 0:128], in1=K[msk][:], op=ALU.mult), reads=[pAb, KB[msk]], writes=[ATb])
                qe, qeb = qer.next()
                P.op("act", lambda e, qe=qe, bT=bT: e.activation(out=qe[:], in_=bT[:], func=AF.Exp), reads=[bTb], writes=[qeb])
                P.op("pool", lambda e, qe=qe, qs=qs: e.tensor_tensor(out=qe[:], in0=qe[:], in1=qs[:], op=ALU.mult), reads=[qeb, qsb], writes=[qeb])
                bl = bT[:, 127:128] if dr == 0 else bT[:, 0:1]
                kd, kdb = kdr.next()
                P.op("act", lambda e, kd=kd, bT=bT, bl=bl: e.activation(out=kd[:], in_=bT[:], func=AF.Exp, scale=-1.0, bias=bl), reads=[bTb], writes=[kdb])
                P.op("pool", lambda e, kd=kd, kT=kT: e.tensor_tensor(out=kd[:], in0=kd[:], in1=kT[:], op=ALU.mult), reads=[kdb, kTb], writes=[kdb])
                pK, pKb = psK.next()
                P.op("pe", lambda e, pK=pK, kd=kd: e.transpose(pK[:, 0:128], kd[:], K["ident"][:]), reads=[kdb, KB["ident"]], writes=[pKb])
                Kd, Kdb = Kdr.next()
                P.op("act", lambda e, Kd=Kd, pK=pK: e.copy(out=Kd[:], in_=pK[:, 0:128]), reads=[pKb], writes=[Kdb])
                sm, smb = smr.next()
                P.op("act", lambda e, sm=sm, bl=bl: e.activation(out=sm[:, 0:1], in_=bl, func=AF.Exp), reads=[bTb], writes=[smb])
                if _CUT == 4:
                    continue
                s, sb_ = S[dr, h], Sb[dr, h]
                pO, pOb = psO.next()
                P.op("pe", lambda e, pO=pO, qe=qe, s=s: e.matmul(pO[:, 0:128], lhsT=qe[:], rhs=s[:], start=True, stop=False), reads=[qeb, sb_], writes=[pOb])
                P.op("pe", lambda e, pO=pO, AT=AT, vi=vi: e.matmul(pO[:, 0:128], lhsT=AT[:], rhs=vi[:], start=False, stop=True), reads=[ATb, vib], writes=[pOb], skip_same=True)
                osl = Oacc[:, c, h * 128:(h + 1) * 128]
                P.op("dve", lambda e, osl=osl, pO=pO: e.tensor_tensor(out=osl, in0=osl, in1=pO[:, 0:128], op=ALU.add), reads=[pOb, ob[c]], writes=[ob[c]])
                pS, pSb = psS.next()
                P.op("pe", lambda e, pS=pS, Kd=Kd, vi=vi: e.matmul(pS[:, 0:128], lhsT=Kd[:], rhs=vi[:], start=True, stop=True), reads=[Kdb, vib], writes=[pSb])
                P.op("dve", lambda e, s=s, sm=sm, pS=pS: e.scalar_tensor_tensor(out=s[:], in0=s[:], scalar=sm[:, 0:1], in1=pS[:, 0:128], op0=ALU.mult, op1=ALU.add),
                     reads=[sb_, smb, pSb], writes=[sb_])
    if not os.environ.get('HG_NOFIN'):
        _finish_gated(C, P, T, Oacc, ob, TM, TM_CG, nw, nwb, yout)


def phase_gdn(nc, C, P, K, KB, T, FM, TM, X, yout, zero, zb, TM2, FMn):
    NT = T // 128
    psA = C.ring(4, [128, 512], psum=True)
    psB = C.ring(4, [128, 512], psum=True)
    Oacc = C.sb([128, NT, 256]); ob = [Buf() for _ in range(NT)]
    for c in range(NT):
        P.op("pool", lambda e, c=c: e.memset(Oacc[:, c, :], 0.0), writes=[ob[c]])
    convw = C.sb([128, 5, 6]); cwb = Buf()
    P.dma("sp", convw[:], X["convw"][:, :, :], writes=[cwb])
    ga = C.sb([128, 4]); gdt = C.sb([128, 4]); nw = C.sb([128, 128]); gab, gdb, nwb = Buf(), Buf(), Buf()
    P.dma("sp", ga[:], X["ga"][:, :], writes=[gab])
    P.dma("sp", gdt[:], X["gdt"][:, :], writes=[gdb])
    P.dma("sp", nw[:], X["gnw"][:, :], writes=[nwb])
    diag = C.sb([128, 30, 128]); dgb = Buf()
    for ft in range(6):
        for k in range(5):
            P.op("pool" if (ft + k) % 2 else "dve", lambda e, ft=ft, k=k: e.tensor_scalar(out=diag[:, ft * 5 + k, :], in0=K["ident"][:], scalar1=convw[:, k, ft:ft + 1], scalar2=None, op0=ALU.mult),
                 reads=[KB["ident"], cwb], writes=[dgb], skip_same=True)

    raw = C.sb([128, NT, 8]); rawb = Buf()
    tmv = TM.rearrange("(nt p) c -> p nt c", p=128)
    for a in range(0, NT, 8):
        b = min(NT, a + 8)
        P.dma("sp", raw[:, a:b, :], tmv[:, a:b, 0:8], writes=[rawb])
    beta = C.sb([128, NT, 4]); btb = Buf()
    P.op("act", lambda e: e.activation(out=beta[:], in_=raw[:, :, 0:4], func=AF.Exp, scale=-1.0), reads=[rawb], writes=[btb])
    P.op("dve", lambda e: e.tensor_scalar_add(out=beta[:], in0=beta[:], scalar1=1.0), reads=[btb], writes=[btb])
    P.op("dve", lambda e: e.reciprocal(out=beta[:], in_=beta[:]), reads=[btb], writes=[btb])
    xs = C.sb([128, NT, 4]); xsb = Buf()
    P.op("dve", lambda e: e.tensor_tensor(out=xs[:], in0=raw[:, :, 4:8], in1=gdt[:].unsqueeze(1).to_broadcast([128, NT, 4]), op=ALU.add), reads=[rawb, gdb], writes=[xsb])
    ax = C.sb([128, NT, 4]); axb = Buf()
    P.op("dve", lambda e: e.tensor_scalar(out=ax[:], in0=xs[:], scalar1=-1.0, scalar2=None, op0=ALU.mult), reads=[xsb], writes=[axb])
    P.op("dve", lambda e: e.tensor_tensor(out=ax[:], in0=ax[:], in1=xs[:], op=ALU.max), reads=[xsb, axb], writes=[axb])
    P.op("act", lambda e: e.activation(out=ax[:], in_=ax[:], func=AF.Exp, scale=-1.0), reads=[axb], writes=[axb])
    P.op("act", lambda e: e.activation(out=ax[:], in_=ax[:], func=AF.Ln, bias=1.0), reads=[axb], writes=[axb])
    P.op("dve", lambda e: e.tensor_scalar_max(out=xs[:], in0=xs[:], scalar1=0.0), reads=[xsb], writes=[xsb])
    P.op("dve", lambda e: e.tensor_tensor(out=xs[:], in0=xs[:], in1=ax[:], op=ALU.add), reads=[xsb, axb], writes=[xsb])
    ea = C.sb([128, 4]); eab = Buf()
    P.op("act", lambda e: e.activation(out=ea[:], in_=ga[:], func=AF.Exp), reads=[gab], writes=[eab])
    G = C.sb([128, NT, 4]); Gb = Buf()
    P.op("dve", lambda e: e.scalar_tensor_tensor(out=G[:], in0=xs[:], scalar=-1.0, in1=ea[:].unsqueeze(1).to_broadcast([128, NT, 4]), op0=ALU.mult, op1=ALU.mult),
         reads=[xsb, eab], writes=[Gb])
    Gd, gcum, egl, bxe, ekd = {}, {}, {}, {}, {}
    Gdb, gcb, eglb, bxb, ekb = {}, {}, {}, {}, {}
    for dr in range(2):
        Gd[dr] = C.sb([128, NT, 2]); Gdb[dr] = Buf()
        P.op("dve", lambda e, dr=dr: e.tensor_copy(out=Gd[dr][:], in_=G[:, :, 2 * dr:2 * dr + 2]), reads=[Gb], writes=[Gdb[dr]])
        cum = "ucum" if dr == 0 else "lcum"
        p, pb = psA.next()
        P.op("pe", lambda e, p=p, dr=dr, cum=cum: e.matmul(p[:, 0:2 * NT], lhsT=K[cum][:], rhs=Gd[dr][:].rearrange("p n h -> p (n h)"), start=True, stop=True),
             reads=[KB[cum], Gdb[dr]], writes=[pb])
        gcum[dr] = C.sb([128, NT, 2]); gcb[dr] = Buf()
        P.op("dve", lambda e, p=p, dr=dr: e.tensor_copy(out=gcum[dr][:].rearrange("p n h -> p (n h)"), in_=p[:, 0:2 * NT]), reads=[pb], writes=[gcb[dr]])
        p, pb = psA.next()
        P.op("pe", lambda e, p=p, dr=dr: e.matmul(p[:, 0:2 * NT], lhsT=K["ones"][:], rhs=Gd[dr][:].rearrange("p n h -> p (n h)"), start=True, stop=True),
             reads=[KB["ones"], Gdb[dr]], writes=[pb])
        gl = C.sb([128, NT, 2]); glb = Buf()
        P.op("dve", lambda e, p=p, gl=gl: e.tensor_copy(out=gl[:].rearrange("p n h -> p (n h)"), in_=p[:, 0:2 * NT]), reads=[pb], writes=[glb])
        egl[dr] = C.sb([128, NT, 2]); eglb[dr] = Buf()
        P.op("act", lambda e, dr=dr, gl=gl: e.activation(out=egl[dr][:], in_=gl[:], func=AF.Exp), reads=[glb], writes=[eglb[dr]])
        bxe[dr] = C.sb([128, NT, 2]); bxb[dr] = Buf()
        P.op("act", lambda e, dr=dr: e.activation(out=bxe[dr][:], in_=gcum[dr][:], func=AF.Exp), reads=[gcb[dr]], writes=[bxb[dr]])
        P.op("dve", lambda e, dr=dr: e.tensor_tensor(out=bxe[dr][:], in0=bxe[dr][:], in1=beta[:, :, 2 * dr:2 * dr + 2], op=ALU.mult), reads=[bxb[dr], btb], writes=[bxb[dr]])
        ekd[dr] = C.sb([128, NT, 2]); ekb[dr] = Buf()
        P.op("dve", lambda e, dr=dr, gl=gl: e.tensor_tensor(out=ekd[dr][:], in0=gl[:], in1=gcum[dr][:], op=ALU.subtract), reads=[glb, gcb[dr]], writes=[ekb[dr]])
        P.op("act", lambda e, dr=dr: e.activation(out=ekd[dr][:], in_=ekd[dr][:], func=AF.Exp), reads=[ekb[dr]], writes=[ekb[dr]])

    xwr = C.ring(3, [128, 516]); sr = C.ring(3, [128, 512]); er = C.ring(2, [128, 512]); sqr = C.ring(2, [128, 512]); rnr = C.ring(2, [128, 512])
    tmr = C.ring(2, [128, 4, 128])
    groups = [(0, 256, 0, 256)] + [(a, min(a + 512, T), 256, T) for a in range(256, T, 512)]
    tiles6 = ["gq0", "gq1", "gk0", "gk1", "gv0", "gv1"]
    for (a, b, s0, s1) in groups:
        n = b - a
        for ft, nm in enumerate(tiles6):
            xw, xwb = xwr.next()
            lo, hi = max(a - 2, s0), min(b + 2, s1)
            if lo > a - 2:
                P.op("pool", lambda e, xw=xw: e.memset(xw[:, 0:2], 0.0), writes=[xwb])
            if hi < b + 2:
                P.op("pool", lambda e, xw=xw, n=n: e.memset(xw[:, n + 2:n + 4], 0.0), writes=[xwb])
            P.dma("sp", xw[:, lo - (a - 2):hi - (a - 2)], FM[FM_IDX[nm], :, lo:hi], writes=[xwb])
            p, pb = psA.next()
            for k in range(5):
                P.op("pe", lambda e, p=p, xw=xw, k=k, ft=ft, n=n: e.matmul(p[:, 0:n], lhsT=diag[:, ft * 5 + k, :], rhs=xw[:, k:k + n], start=(k == 0), stop=(k == 4)),
                     reads=[dgb, xwb], writes=[pb], skip_same=True)
            ex, exb = er.next()
            P.op("act", lambda e, ex=ex, p=p, n=n: e.activation(out=ex[:, 0:n], in_=p[:, 0:n], func=AF.Exp, scale=-1.0), reads=[pb], writes=[exb])
            P.op("pool", lambda e, ex=ex, n=n: e.tensor_scalar_add(out=ex[:, 0:n], in0=ex[:, 0:n], scalar1=1.0), reads=[exb], writes=[exb])
            P.op("dve", lambda e, ex=ex, n=n: e.reciprocal(out=ex[:, 0:n], in_=ex[:, 0:n]), reads=[exb], writes=[exb])
            s, sb_ = sr.next()
            P.op("dve", lambda e, s=s, ex=ex, p=p, n=n: e.tensor_tensor(out=s[:, 0:n], in0=p[:, 0:n], in1=ex[:, 0:n], op=ALU.mult), reads=[pb, exb], writes=[sb_])
            if ft < 4:
                sq, sqb = sqr.next()
                P.op("pool", lambda e, sq=sq, s=s, n=n: e.tensor_tensor(out=sq[:, 0:n], in0=s[:, 0:n], in1=s[:, 0:n], op=ALU.mult), reads=[sb_], writes=[sqb])
                p2, p2b = psA.next()
                P.op("pe", lambda e, p2=p2, sq=sq, n=n: e.matmul(p2[:, 0:n], lhsT=K["ones"][:], rhs=sq[:, 0:n], start=True, stop=True), reads=[KB["ones"], sqb], writes=[p2b])
                rn, rnb = rnr.next()
                P.op("act", lambda e, rn=rn, p2=p2, n=n: e.activation(out=rn[:, 0:n], in_=p2[:, 0:n], func=AF.Ln, bias=1e-6), reads=[p2b], writes=[rnb])
                P.op("act", lambda e, rn=rn, n=n: e.activation(out=rn[:, 0:n], in_=rn[:, 0:n], func=AF.Exp, scale=-0.5), reads=[rnb], writes=[rnb])
                sc = (128.0 ** -0.5) if ft < 2 else 1.0
                P.op("dve", lambda e, s=s, rn=rn, n=n, sc=sc: e.scalar_tensor_tensor(out=s[:, 0:n], in0=s[:, 0:n], scalar=sc, in1=rn[:, 0:n], op0=ALU.mult, op1=ALU.mult),
                     reads=[sb_, rnb], writes=[sb_])
                P.dma("sp", FMn[ft, :, a:b], s[:, 0:n], reads=[sb_])
            if ft >= 2:
                p3, p3b = psA.next()
                for j in range(n // 128):
                    P.op("pe", lambda e, p3=p3, s=s, j=j: e.transpose(p3[:, j * 128:(j + 1) * 128], s[:, j * 128:(j + 1) * 128], K["ident"][:]), reads=[sb_, KB["ident"]], writes=[p3b], skip_same=True)
                tm, tmb = tmr.next()
                P.op("act", lambda e, tm=tm, p3=p3, n=n: e.copy(out=tm[:, 0:n // 128, :].rearrange("p j d -> p (j d)"), in_=p3[:, 0:n]), reads=[p3b], writes=[tmb])
                col = (ft - 2) * 128
                P.dma("sp", TM2[a:b, col:col + 128].rearrange("(j p) d -> p j d", p=128), tm[:, 0:n // 128, :], reads=[tmb])
    P.barrier()

    S, Sb = {}, {}
    for dr in range(2):
        for h in range(2):
            S[dr, h] = C.sb([128, 128]); Sb[dr, h] = Buf()
            P.op("pool", lambda e, s=S[dr, h]: e.memset(s[:], 0.0), writes=[Sb[dr, h]])
    R4 = lambda: C.ring(4, [128, 128])
    ldq, ldk, ldkt, ldv = R4(), R4(), R4(), R4()
    kqr = C.ring(4, [128, 256]); G1r = R4(); Er = R4(); EQr = R4(); qdr = R4(); Ar = R4(); ATr = R4(); MMr = C.ring(4, [128, 256])
    NNr = C.ring(6, [128, 256]); XXr = C.ring(10, [128, 256]); QQr = C.ring(8, [128, 256]); Xr = R4(); nWr = R4(); vbr = R4(); kdr = R4(); vnr = R4()
    fwd, bwd = _chunk_orders(NT)
    for step in range(NT):
        for dr in range(2):
            c = (fwd if dr == 0 else bwd)[step]
            cum, big, strict = ("ucum", "bigU", "strictL") if dr == 0 else ("lcum", "bigL", "strictU")
            cs = slice(c * 128, (c + 1) * 128)
            for h in range(2):
                qT, qTb = ldq.next(); kT, kTb = ldk.next(); kt, ktb = ldkt.next(); vt, vtb = ldv.next()
                P.dma("sp", qT[:], FMn[h, :, cs], writes=[qTb])
                P.dma("sp", kT[:], FMn[2 + h, :, cs], writes=[kTb])
                P.dma("sp", kt[:], TM2[cs, h * 128:(h + 1) * 128], writes=[ktb])
                P.dma("sp", vt[:], TM2[cs, 256 + h * 128:256 + (h + 1) * 128], writes=[vtb])
                p, pb = psA.next()
                P.op("pe", lambda e, p=p, kT=kT: e.matmul(p[:, 0:128], lhsT=kT[:], rhs=kT[:], start=True, stop=True), reads=[kTb], writes=[pb])
                P.op("pe", lambda e, p=p, kT=kT, qT=qT: e.matmul(p[:, 128:256], lhsT=qT[:], rhs=kT[:], start=True, stop=True), reads=[kTb, qTb], writes=[pb], skip_same=True)
                kq, kqb = kqr.next()
                P.op("act", lambda e, kq=kq, p=p: e.copy(out=kq[:], in_=p[:, 0:256]), reads=[pb], writes=[kqb])
                g1, g1b = G1r.next()
                P.op("pool", lambda e, g1=g1, dr=dr, c=c, h=h: e.tensor_scalar(out=g1[:], in0=K["ones"][:], scalar1=Gd[dr][:, c, h:h + 1], scalar2=None, op0=ALU.mult),
                     reads=[KB["ones"], Gdb[dr]], writes=[g1b])
                pg, pgb = psA.next()
                P.op("pe", lambda e, pg=pg, g1=g1, cum=cum: e.matmul(pg[:, 128:256], lhsT=g1[:], rhs=K[cum][:], start=True, stop=True), reads=[g1b, KB[cum]], writes=[pgb])
                P.op("pe", lambda e, pg=pg, g1=g1, cum=cum: e.matmul(pg[:, 0:128], lhsT=g1[:], rhs=K[cum][:], start=True, stop=False), reads=[g1b, KB[cum]], writes=[pgb], skip_same=True)
                P.op("pe", lambda e, pg=pg, big=big: e.matmul(pg[:, 0:128], lhsT=K["ident"][:], rhs=K[big][:], start=False, stop=True), reads=[KB["ident"], KB[big]], writes=[pgb], skip_same=True)
                E, Eb = Er.next()
                P.op("act", lambda e, E=E, pg=pg, dr=dr, c=c, h=h: e.activation(out=E[:], in_=pg[:, 0:128], func=AF.Exp, scale=-1.0, bias=gcum[dr][:, c, h:h + 1]),
                     reads=[pgb, gcb[dr]], writes=[Eb])
                EQ, EQb = EQr.next()
                P.op("act", lambda e, EQ=EQ, pg=pg: e.activation(out=EQ[:], in_=pg[:, 128:256], func=AF.Exp), reads=[pgb], writes=[EQb])
                qd, qdb = qdr.next()
                P.op("pool", lambda e, qd=qd, qT=qT, EQ=EQ: e.tensor_tensor(out=qd[:], in0=qT[:], in1=EQ[:], op=ALU.mult), reads=[qTb, EQb], writes=[qdb])
                MM, MMb = MMr.next()
                P.op("dve", lambda e, MM=MM, kq=kq, E=E, dr=dr, c=c, h=h: e.scalar_tensor_tensor(out=MM[:, 0:128], in0=kq[:, 0:128], scalar=beta[:, c, 2 * dr + h:2 * dr + h + 1], in1=E[:],
                                                                                            op0=ALU.mult, op1=ALU.mult), reads=[kqb, Eb, btb], writes=[MMb])
                P.op("pool", lambda e, MM=MM, strict=strict: e.tensor_tensor(out=MM[:, 0:128], in0=MM[:, 0:128], in1=K[strict][:], op=ALU.mult), reads=[MMb, KB[strict]], writes=[MMb])
                A, Ab = Ar.next()
                P.op("pool", lambda e, A=A, kq=kq, E=E: e.tensor_tensor(out=A[:], in0=kq[:, 128:256], in1=E[:], op=ALU.mult), reads=[kqb, Eb], writes=[Ab])
                pt, ptb = psA.next()
                P.op("pe", lambda e, pt=pt, MM=MM: e.transpose(pt[:, 0:128], MM[:, 0:128], K["ident"][:]), reads=[MMb, KB["ident"]], writes=[ptb])
                P.op("pe", lambda e, pt=pt, A=A: e.transpose(pt[:, 128:256], A[:], K["ident"][:]), reads=[Ab, KB["ident"]], writes=[ptb], skip_same=True)
                P.op("dve", lambda e, MM=MM, pt=pt: e.tensor_copy(out=MM[:, 128:256], in_=pt[:, 0:128]), reads=[ptb], writes=[MMb])
                AT, ATb = ATr.next()
                P.op("act", lambda e, AT=AT, pt=pt: e.copy(out=AT[:], in_=pt[:, 128:256]), reads=[ptb], writes=[ATb])
                ND, NDb = NNr.next()
                P.op("pool", lambda e, ND=ND, MM=MM: e.tensor_tensor(out=ND[:], in0=MM[:], in1=K["bd8"][:], op=ALU.mult), reads=[MMb, KB["bd8"]], writes=[NDb])
                XX, XXb = XXr.next()
                P.op("pool", lambda e, XX=XX, ND=ND: e.tensor_tensor(out=XX[:], in0=K["ii2"][:], in1=ND[:], op=ALU.subtract), reads=[KB["ii2"], NDb], writes=[XXb])
                Qc, Qcb = ND, NDb
                for lev in range(2):
                    pq, pqb = psB.next()
                    P.op("pe", lambda e, pq=pq, Qc=Qc: e.matmul(pq[:, 0:128], lhsT=Qc[:, 128:256], rhs=Qc[:, 0:128], start=True, stop=True), reads=[Qcb], writes=[pqb])
                    P.op("pe", lambda e, pq=pq, Qc=Qc: e.matmul(pq[:, 128:256], lhsT=Qc[:, 0:128], rhs=Qc[:, 128:256], start=True, stop=True), reads=[Qcb], writes=[pqb], skip_same=True)
                    QQ, QQb = QQr.next()
                    P.op("act", lambda e, QQ=QQ, pq=pq: e.copy(out=QQ[:], in_=pq[:, 0:256]), reads=[pqb], writes=[QQb])
                    p2, p2b = psB.next()
                    P.op("pe", lambda e, p2=p2, QQ=QQ, XX=XX: e.matmul(p2[:, 0:128], lhsT=QQ[:, 128:256], rhs=XX[:, 0:128], start=True, stop=True), reads=[QQb, XXb], writes=[p2b])
                    P.op("pe", lambda e, p2=p2, QQ=QQ, XX=XX: e.matmul(p2[:, 128:256], lhsT=QQ[:, 0:128], rhs=XX[:, 128:256], start=True, stop=True), reads=[QQb, XXb], writes=[p2b], skip_same=True)
                    XX2, XX2b = XXr.next()
                    P.op("dve", lambda e, XX2=XX2, XX=XX, p2=p2: e.tensor_tensor(out=XX2[:], in0=XX[:], in1=p2[:, 0:256], op=ALU.add), reads=[XXb, p2b], writes=[XX2b])
                    XX, XXb = XX2, XX2b
                    Qc, Qcb = QQ, QQb
                for s_ in (8, 16, 32, 64):
                    last = s_ == 64
                    NN, NNb = NNr.next()
                    P.op("pool", lambda e, NN=NN, MM=MM, s_=s_: e.tensor_tensor(out=NN[:], in0=MM[:], in1=K["ms%d" % s_][:], op=ALU.mult), reads=[MMb, KB["ms%d" % s_]], writes=[NNb])
                    py, pyb = psB.next()
                    if not last:
                        P.op("pe", lambda e, py=py, XX=XX, NN=NN: e.matmul(py[:, 0:128], lhsT=XX[:, 128:256], rhs=NN[:, 0:128], start=True, stop=True), reads=[XXb, NNb], writes=[pyb])
                    P.op("pe", lambda e, py=py, XX=XX, NN=NN: e.matmul(py[:, 128:256], lhsT=NN[:, 0:128], rhs=XX[:, 128:256], start=True, stop=True), reads=[XXb, NNb], writes=[pyb], skip_same=not last)
                    YY, YYb = QQr.next()
                    lo_ = 128 if last else 0
                    P.op("act", lambda e, YY=YY, py=py, lo_=lo_: e.copy(out=YY[:, lo_:256], in_=py[:, lo_:256]), reads=[pyb], writes=[YYb])
                    pz, pzb = psB.next()
                    if not last:
                        P.op("pe", lambda e, pz=pz, YY=YY, XX=XX: e.matmul(pz[:, 0:128], lhsT=YY[:, 128:256], rhs=XX[:, 0:128], start=True, stop=True), reads=[YYb, XXb], writes=[pzb])
                    P.op("pe", lambda e, pz=pz, YY=YY, XX=XX: e.matmul(pz[:, 128:256], lhsT=XX[:, 0:128], rhs=YY[:, 128:256], start=True, stop=True), reads=[YYb, XXb], writes=[pzb], skip_same=not last)
                    XX2, XX2b = XXr.next()
                    P.op("dve", lambda e, XX2=XX2, XX=XX, pz=pz, lo_=lo_: e.tensor_tensor(out=XX2[:, lo_:256], in0=XX[:, lo_:256], in1=pz[:, lo_:256], op=ALU.subtract), reads=[XXb, pzb], writes=[XX2b])
                    XX, XXb = XX2, XX2b
                TT, TTb = XX[:, 128:256], XXb
                Xt, Xb = Xr.next()
                P.op("pool", lambda e, Xt=Xt, kt=kt, dr=dr, c=c, h=h: e.tensor_scalar(out=Xt[:], in0=kt[:], scalar1=bxe[dr][:, c, h:h + 1], scalar2=None, op0=ALU.mult),
                     reads=[ktb, bxb[dr]], writes=[Xb])
                pw, pwb = psB.next()
                P.op("pe", lambda e, pw=pw, Xt=Xt, TT=TT: e.matmul(pw[:, 0:128], lhsT=Xt[:], rhs=TT, start=True, stop=True), reads=[Xb, TTb], writes=[pwb])
                nW, nWb = nWr.next()
                P.op("dve", lambda e, nW=nW, pw=pw: e.tensor_scalar(out=nW[:], in0=pw[:, 0:128], scalar1=-1.0, scalar2=None, op0=ALU.mult), reads=[pwb], writes=[nWb])
                vb_, vbb = vbr.next()
                P.op("pool", lambda e, vb_=vb_, vt=vt, dr=dr, c=c, h=h: e.tensor_scalar(out=vb_[:], in0=vt[:], scalar1=beta[:, c, 2 * dr + h:2 * dr + h + 1], scalar2=None, op0=ALU.mult),
                     reads=[vtb, btb], writes=[vbb])
                kd, kdb = kdr.next()
                P.op("pool", lambda e, kd=kd, kt=kt, dr=dr, c=c, h=h: e.tensor_scalar(out=kd[:], in0=kt[:], scalar1=ekd[dr][:, c, h:h + 1], scalar2=None, op0=ALU.mult),
                     reads=[ktb, ekb[dr]], writes=[kdb])
                s, sb_ = S[dr, h], Sb[dr, h]
                pv, pvb = psB.next()
                P.op("pe", lambda e, pv=pv, TT=TT, vb_=vb_: e.matmul(pv[:, 0:128], lhsT=TT, rhs=vb_[:], start=True, stop=False), reads=[TTb, vbb], writes=[pvb])
                P.op("pe", lambda e, pv=pv, nW=nW, s=s: e.matmul(pv[:, 0:128], lhsT=nW[:], rhs=s[:], start=False, stop=True), reads=[nWb, sb_], writes=[pvb], skip_same=True)
                vn, vnb = vnr.next()
                P.op("act", lambda e, vn=vn, pv=pv: e.copy(out=vn[:], in_=pv[:, 0:128]), reads=[pvb], writes=[vnb])
                po, pob = psB.next()
                P.op("pe", lambda e, po=po, qd=qd, s=s: e.matmul(po[:, 0:128], lhsT=qd[:], rhs=s[:], start=True, stop=False), reads=[qdb, sb_], writes=[pob])
                P.op("pe", lambda e, po=po, AT=AT, vn=vn: e.matmul(po[:, 0:128], lhsT=AT[:], rhs=vn[:], start=False, stop=True), reads=[ATb, vnb], writes=[pob], skip_same=True)
                osl = Oacc[:, c, h * 128:(h + 1) * 128]
                P.op("dve", lambda e, osl=osl, po=po: e.tensor_tensor(out=osl, in0=osl, in1=po[:, 0:128], op=ALU.add), reads=[pob, ob[c]], writes=[ob[c]])
                pS, pSb = psB.next()
                P.op("pe", lambda e, pS=pS, kd=kd, vn=vn: e.matmul(pS[:, 0:128], lhsT=kd[:], rhs=vn[:], start=True, stop=True), reads=[kdb, vnb], writes=[pSb])
                P.op("dve", lambda e, s=s, pS=pS, dr=dr, c=c, h=h: e.scalar_tensor_tensor(out=s[:], in0=s[:], scalar=egl[dr][:, c, h:h + 1], in1=pS[:, 0:128], op0=ALU.mult, op1=ALU.add),
                     reads=[sb_, eglb[dr], pSb], writes=[sb_])
    _finish_gated(C, P, T, Oacc, ob, TM, TM_GA, nw, nwb, yout)


def gdn_params(hh, conv, a_log, dtb, nw):
    cw = np.zeros((128, 5, 6), np.float32)
    for i, base in enumerate((0, 512, 1024)):
        for h in range(2):
            cols = base + (2 * hh + h) * 128 + np.arange(128)
            cw[:, :, i * 2 + h] = conv[:, cols].T
    sel = np.array([[dr, 2 * hh + h] for dr in range(2) for h in range(2)])
    ga = np.broadcast_to(a_log[sel[:, 0], sel[:, 1]][None], (128, 4)).astype(np.float32).copy()
    gdt = np.broadcast_to(dtb[sel[:, 0], sel[:, 1]][None], (128, 4)).astype(np.float32).copy()
    return {"convw": cw, "ga": ga, "gdt": gdt, "gnw": np.broadcast_to(nw[None], (128, 128)).astype(np.float32).copy()}


ALPHA = (2 * DEPTH) ** 0.25
GT = 3


def _layer_norm(P, C, rings, t, tb, grow, gb, brow, bb, out, outb, eps=1e-5):
    st, stb = rings["st"].next()
    P.op("dve", lambda e: e.reduce_sum(out=st[:, 0:1], in_=t[:], axis=AX.X), reads=[tb], writes=[stb])
    P.op("dve", lambda e: e.tensor_scalar(out=st[:, 1:2], in0=st[:, 0:1], scalar1=-1.0 / D, scalar2=None, op0=ALU.mult), reads=[stb], writes=[stb])
    P.op("dve", lambda e: e.tensor_scalar(out=t[:], in0=t[:], scalar1=st[:, 1:2], scalar2=None, op0=ALU.add), reads=[tb, stb], writes=[tb])
    sq, sqb = rings["sq"].next()
    P.op("pool", lambda e: e.tensor_tensor(out=sq[:], in0=t[:], in1=t[:], op=ALU.mult), reads=[tb], writes=[sqb])
    P.op("dve", lambda e: e.reduce_sum(out=st[:, 2:3], in_=sq[:], axis=AX.X), reads=[sqb], writes=[stb])
    P.op("dve", lambda e: e.tensor_scalar(out=st[:, 3:4], in0=st[:, 2:3], scalar1=1.0 / D, scalar2=eps, op0=ALU.mult, op1=ALU.add), reads=[stb], writes=[stb])
    P.op("act", lambda e: e.activation(out=st[:, 3:4], in_=st[:, 3:4], func=AF.Ln), reads=[stb], writes=[stb])
    P.op("act", lambda e: e.activation(out=st[:, 4:5], in_=st[:, 3:4], func=AF.Exp, scale=-0.5), reads=[stb], writes=[stb])
    P.op("dve", lambda e: e.scalar_tensor_tensor(out=t[:], in0=t[:], scalar=st[:, 4:5], in1=grow[:], op0=ALU.mult, op1=ALU.mult), reads=[tb, stb, gb], writes=[tb])
    P.op("pool", lambda e: e.tensor_tensor(out=out[:], in0=t[:], in1=brow[:], op=ALU.add), reads=[tb, bb], writes=[outb])


def build_B(ntok, debug=False):
    NTL = ntok // 128
    assert NTL % GT == 0
    NG = NTL // GT
    nc = bass.Bass("TRN2", target_bir_lowering=False)
    din = lambda n, s, dt=F32: nc.dram_tensor(n, list(s), dt, kind="ExternalInput").ap()
    xin = din("xin", [ntok, D])
    y3 = din("y3", [ntok, 1536])
    c2 = din("c2", [128, 8, 2])
    wmod = din("wmod", [D, 6144])
    bmod = din("bmod", [128, 48])
    wg = din("wg", [D, 3072])
    wbr = din("wbr", [3, 512, D])
    wo = din("wo", [D, D])
    lnrows = din("lnrows", [4, 128, D])
    wr = din("wr", [D, 36])
    brr = din("brr", [128, 36])
    w1 = din("w1", [32, D, 512])
    w3 = din("w3", [32, D, 512])
    w2 = din("w2", [32, 512, D])
    cn = {k: din("k_" + k, [128, 128]) for k in ("ident", "ones")}
    xout = nc.dram_tensor("xout", [ntok, D], F32, kind="ExternalOutput").ap()
    x1d = nc.dram_tensor("x1d", [ntok, D], F32, kind="Internal").ap()
    h2d = nc.dram_tensor("h2d", [NTL, 128, 8, 128], BF16, kind="Internal").ap()
    with ExitStack() as es0:
        P = Prog(nc, es0)
        C0 = Ctx(nc, es0, P)
        K, KB = {}, {}
        for k, ap in cn.items():
            K[k] = C0.sb([128, 128], name="sk_" + k); KB[k] = Buf()
            P.dma("sp", K[k][:], ap[:, :], writes=[KB[k]])
        g12 = C0.sb([128, 2, 2, D]); g12b = Buf()
        Wt = C0.sb([128, NTL, 32]); Wtb = [Buf() for _ in range(NTL)]
        modT = C0.sb([128, 48, 2]); modb = Buf()
        with ExitStack() as es:
            C = Ctx(nc, es, P)
            csb = C.sb([128, 8, 2]); cb = Buf()
            P.dma("sp", csb[:], c2[:, :, :], writes=[cb])
            csl = C.sb([128, 8, 2]); cslb = Buf()
            P.op("act", lambda e: e.activation(out=csl[:], in_=csb[:], func=AF.Silu), reads=[cb], writes=[cslb])
            bm = C.sb([128, 48]); bmb = Buf()
            P.dma("sp", bm[:], bmod[:, :], writes=[bmb])
            wm = C.ring(2, [128, 2048])
            pm = C.ps([128, 512]); pmb = Buf(excl=True)
            wmv = wmod.rearrange("(kt p) c -> kt p c", p=128)
            for blk in range(3):
                P.op("dve", lambda e: e.memset(pm[:, 0:32], 0.0), writes=[pmb])
                for kt in range(8):
                    w, wb = wm.next()
                    P.dma("sp" if kt % 2 == 0 else "pool", w[:], wmv[kt][:, blk * 2048:(blk + 1) * 2048], writes=[wb])
                    for j in range(16):
                        P.op("pe", lambda e, w=w, j=j, kt=kt: e.matmul(pm[:, 2 * j:2 * j + 2], lhsT=w[:, j * 128:(j + 1) * 128], rhs=csl[:, kt, :],
                                                                       start=False, stop=(kt == 7), skip_group_check=True), reads=[wb, cslb], writes=[pmb], skip_same=True)
                P.op("dve", lambda e, blk=blk: e.tensor_tensor(out=modT[:, blk * 16:(blk + 1) * 16, :], in0=pm[:, 0:32].rearrange("p (j s) -> p j s", s=2),
                                                              in1=bm[:, blk * 16:(blk + 1) * 16].unsqueeze(2).to_broadcast([128, 16, 2]), op=ALU.add), reads=[pmb, bmb], writes=[modb])
            for a in (8, 32):
                P.op("dve", lambda e, a=a: e.tensor_scalar_add(out=modT[:, a:a + 8, :], in0=modT[:, a:a + 8, :], scalar1=1.0), reads=[modb], writes=[modb])
            dgr = C.ring(2, [128, 128]); pbr = C.ring(2, [128, 512], psum=True)
            for gi, base in enumerate((16, 40)):
                for s in range(2):
                    for half in range(2):
                        pb_, pbb = pbr.next()
                        for j in range(4):
                            dg, dgb = dgr.next()
                            P.op("dve", lambda e, dg=dg, base=base, j=j, half=half, s=s: e.tensor_scalar(out=dg[:], in0=K["ident"][:], scalar1=modT[:, base + half * 4 + j, s:s + 1], scalar2=None, op0=ALU.mult),
                                 reads=[KB["ident"], modb], writes=[dgb])
                            P.op("pe", lambda e, pb_=pb_, dg=dg, j=j: e.matmul(pb_[:, j * 128:(j + 1) * 128], lhsT=K["ones"][:], rhs=dg[:], start=True, stop=True),
                                 reads=[KB["ones"], dgb], writes=[pbb])
                        P.op("act", lambda e, pb_=pb_, gi=gi, s=s, half=half: e.copy(out=g12[:, gi, s, half * 512:(half + 1) * 512], in_=pb_[:, 0:512]), reads=[pbb], writes=[g12b])
            P.barrier()
            P.emit()
        with ExitStack() as es:
            C = Ctx(nc, es, P)
            rows = C.sb([128, 4, D]); rowb = Buf()
            for i in range(2):
                P.dma("sp", rows[:, i, :], lnrows[i], writes=[rowb])
            Wg = C.sb([128, 8, 3072], BF16); Wgb = Buf()
            Wb = C.sb([128, 12, D], BF16); Wbb = Buf()
            Wo = C.sb([128, 8, D], BF16); Wob = Buf()
            Wr = C.sb([128, 8, 36]); Wrb = Buf()
            brs = C.sb([128, 36]); brb = Buf()
            P.dma("sp", brs[:], brr[:, :], writes=[brb])
            wgv = wg.rearrange("(kt p) c -> kt p c", p=128); wov = wo.rearrange("(kt p) c -> kt p c", p=128); wrv = wr.rearrange("(kt p) c -> kt p c", p=128)
            for kt in range(8):
                P.dma("pool", Wg[:, kt, :], wgv[kt], writes=[Wgb])
                P.dma("pool", Wo[:, kt, :], wov[kt], writes=[Wob])
                P.dma("sp", Wr[:, kt, :], wrv[kt], writes=[Wrb])
            for br in range(3):
                wv = wbr[br].rearrange("(kt p) c -> kt p c", p=128)
                for kt in range(4):
                    P.dma("pool", Wb[:, br * 4 + kt, :], wv[kt], writes=[Wbb])
            xr = C.ring(2, [128, D]); y3r = C.ring(1, [128, 1536]); hr = C.ring(2, [128, 8, 128], BF16); yTr = C.ring(1, [128, 12, 128], BF16)
            gater = C.ring(1, [128, 3072]); mr = C.ring(1, [128, D]); mTr = C.ring(1, [128, 8, 128], BF16); tr_ = C.ring(1, [128, D]); tmpr = C.ring(2, [128, 512])
            x1r = C.ring(1, [128, D]); h32r = C.ring(1, [128, 8, 128]); h2r = C.ring(2, [128, 8, 128], BF16)
            rings = {"st": C.ring(2, [128, 8]), "sq": C.ring(1, [128, D])}
            lgr = C.ring(2, [128, 64]); smr = C.ring(2, [128, 16]); elr = C.ring(2, [128, 8]); m8r = C.ring(2, [128, 8])
            ptr = C.ring(2, [128, 512], psum=True); pfr = C.ring(3, [128, 512], psum=True)
            ev = [0]

            def transpose_mod(src, srcb, dst, dstb, sc_base, sh_base, s, dst32=None, dst32b=None):
                for half in range(2):
                    pt, ptb = ptr.next()
                    for j in range(4):
                        kt = half * 4 + j
                        P.op("pe", lambda e, pt=pt, j=j, kt=kt: e.transpose(pt[:, j * 128:(j + 1) * 128], src[:, kt * 128:(kt + 1) * 128], K["ident"][:]),
                             reads=[srcb, KB["ident"]], writes=[ptb], skip_same=True)
                    for j in range(4):
                        kt = half * 4 + j
                        o = dst32 if dst32 is not None else dst
                        ob_ = dst32b if dst32 is not None else dstb
                        ev[0] += 1
                        if ev[0] % 2:
                            P.op("dve", lambda e, pt=pt, j=j, kt=kt, o=o: e.tensor_scalar(out=o[:, kt, :], in0=pt[:, j * 128:(j + 1) * 128], scalar1=modT[:, sc_base + kt, s:s + 1],
                                                                                         scalar2=modT[:, sh_base + kt, s:s + 1], op0=ALU.mult, op1=ALU.add), reads=[ptb, modb], writes=[ob_], skip_same=True)
                        else:
                            P.op("act", lambda e, pt=pt, j=j, kt=kt, o=o: e.activation(out=o[:, kt, :], in_=pt[:, j * 128:(j + 1) * 128], func=AF.Identity, scale=modT[:, sc_base + kt, s:s + 1],
                                                                                      bias=modT[:, sh_base + kt, s:s + 1]), reads=[ptb, modb], writes=[ob_], skip_same=True)
                if dst32 is not None:
                    P.op("pool", lambda e: e.tensor_copy(out=dst[:], in_=dst32[:]), reads=[dst32b], writes=[dstb])

            for tt in range(NTL):
                s = 0 if tt < 2 else 1
                x, xb = xr.next()
                P.dma("sp", x[:], xin[tt * 128:(tt + 1) * 128, :], writes=[xb])
                yy, yyb = y3r.next()
                P.dma("sp", yy[:], y3[tt * 128:(tt + 1) * 128, :], writes=[yyb])
                hT, hb = hr.next()
                transpose_mod(x, xb, hT, hb, 8, 0, s)
                gate, gtb = gater.next()
                for ch in range(6):
                    pf, pfb = pfr.next()
                    for kt in range(8):
                        P.op("pe", lambda e, pf=pf, kt=kt, ch=ch, hT=hT: e.matmul(pf[:, 0:512], lhsT=hT[:, kt, :], rhs=Wg[:, kt, ch * 512:(ch + 1) * 512], start=(kt == 0), stop=(kt == 7)),
                             reads=[hb, Wgb], writes=[pfb], skip_same=True)
                    P.op("act", lambda e, pf=pf, ch=ch, gate=gate: e.activation(out=gate[:, ch * 512:(ch + 1) * 512], in_=pf[:, 0:512], func=AF.Exp, scale=-1.0), reads=[pfb], writes=[gtb], skip_same=True)
                P.op("pool", lambda e, gate=gate: e.tensor_scalar_add(out=gate[:], in0=gate[:], scalar1=1.0), reads=[gtb], writes=[gtb])
                P.op("dve", lambda e, gate=gate: e.reciprocal(out=gate[:], in_=gate[:]), reads=[gtb], writes=[gtb])
                yT, yTb = yTr.next()
                for q4 in range(3):
                    pt, ptb = ptr.next()
                    for j in range(4):
                        P.op("pe", lambda e, pt=pt, j=j, q4=q4, yy=yy: e.transpose(pt[:, j * 128:(j + 1) * 128], yy[:, (q4 * 4 + j) * 128:(q4 * 4 + j + 1) * 128], K["ident"][:]),
                             reads=[yyb, KB["ident"]], writes=[ptb], skip_same=True)
                    P.op("act" if q4 % 2 else "dve", (lambda e, pt=pt, q4=q4, yT=yT: e.copy(out=yT[:, q4 * 4:(q4 + 1) * 4, :].rearrange("p j t -> p (j t)"), in_=pt[:, 0:512])) if q4 % 2 else
                         (lambda e, pt=pt, q4=q4, yT=yT: e.tensor_copy(out=yT[:, q4 * 4:(q4 + 1) * 4, :].rearrange("p j t -> p (j t)"), in_=pt[:, 0:512])), reads=[ptb], writes=[yTb], skip_same=True)
                mg, mgb = mr.next()
                for ch in range(2):
                    for br in range(3):
                        pf, pfb = pfr.next()
                        for kt in range(4):
                            P.op("pe", lambda e, pf=pf, kt=kt, br=br, ch=ch, yT=yT: e.matmul(pf[:, 0:512], lhsT=yT[:, br * 4 + kt, :], rhs=Wb[:, br * 4 + kt, ch * 512:(ch + 1) * 512],
                                                                                          start=(kt == 0), stop=(kt == 3)), reads=[yTb, Wbb], writes=[pfb], skip_same=True)
                        gs = gate[:, br * 1024 + ch * 512:br * 1024 + (ch + 1) * 512]
                        if br == 0:
                            P.op("dve", lambda e, pf=pf, gs=gs, mg=mg, ch=ch: e.tensor_tensor(out=mg[:, ch * 512:(ch + 1) * 512], in0=pf[:, 0:512], in1=gs, op=ALU.mult), reads=[pfb, gtb], writes=[mgb])
                        else:
                            tmp, tmpb = tmpr.next()
                            P.op("dve", lambda e, pf=pf, gs=gs, tmp=tmp: e.tensor_tensor(out=tmp[:], in0=pf[:, 0:512], in1=gs, op=ALU.mult), reads=[pfb, gtb], writes=[tmpb])
                            P.op("pool", lambda e, mg=mg, tmp=tmp, ch=ch: e.tensor_tensor(out=mg[:, ch * 512:(ch + 1) * 512], in0=mg[:, ch * 512:(ch + 1) * 512], in1=tmp[:], op=ALU.add), reads=[mgb, tmpb], writes=[mgb])
                mT, mTb = mTr.next()
                for half in range(2):
                    pt, ptb = ptr.next()
                    for j in range(4):
                        kt = half * 4 + j
                        P.op("pe", lambda e, pt=pt, j=j, kt=kt, mg=mg: e.transpose(pt[:, j * 128:(j + 1) * 128], mg[:, kt * 128:(kt + 1) * 128], K["ident"][:]), reads=[mgb, KB["ident"]], writes=[ptb], skip_same=True)
                    P.op("act" if half else "dve", (lambda e, pt=pt, half=half, mT=mT: e.copy(out=mT[:, half * 4:(half + 1) * 4, :].rearrange("p j t -> p (j t)"), in_=pt[:, 0:512])) if half else
                         (lambda e, pt=pt, half=half, mT=mT: e.tensor_copy(out=mT[:, half * 4:(half + 1) * 4, :].rearrange("p j t -> p (j t)"), in_=pt[:, 0:512])), reads=[ptb], writes=[mTb], skip_same=True)
                t, tb = tr_.next()
                for ch in range(2):
                    pf, pfb = pfr.next()
                    for kt in range(8):
                        P.op("pe", lambda e, pf=pf, kt=kt, ch=ch, mT=mT: e.matmul(pf[:, 0:512], lhsT=mT[:, kt, :], rhs=Wo[:, kt, ch * 512:(ch + 1) * 512], start=(kt == 0), stop=(kt == 7)),
                             reads=[mTb, Wob], writes=[pfb], skip_same=True)
                    tmp, tmpb = tmpr.next()
                    P.op("dve", lambda e, pf=pf, tmp=tmp, ch=ch, s=s: e.tensor_tensor(out=tmp[:], in0=pf[:, 0:512], in1=g12[:, 0, s, ch * 512:(ch + 1) * 512], op=ALU.mult), reads=[pfb, g12b], writes=[tmpb])
                    P.op("dve", lambda e, t=t, x=x, tmp=tmp, ch=ch: e.scalar_tensor_tensor(out=t[:, ch * 512:(ch + 1) * 512], in0=x[:, ch * 512:(ch + 1) * 512], scalar=ALPHA, in1=tmp[:], op0=ALU.mult, op1=ALU.add),
                         reads=[xb, tmpb], writes=[tb])
                x1, x1b = x1r.next()
                _layer_norm(P, C, rings, t, tb, rows[:, 0, :], rowb, rows[:, 1, :], rowb, x1, x1b)
                P.dma("sp", x1d[tt * 128:(tt + 1) * 128, :], x1[:], reads=[x1b])
                h32, h32b = h32r.next(); h2, h2b = h2r.next()
                transpose_mod(x1, x1b, h2, h2b, 32, 24, s, dst32=h32, dst32b=h32b)
                P.dma("sp", h2d[tt], h2[:], reads=[h2b])
                pf, pfb = pfr.next()
                for kt in range(8):
                    P.op("pe", lambda e, pf=pf, kt=kt, h32=h32: e.matmul(pf[:, 0:36], lhsT=h32[:, kt, :], rhs=Wr[:, kt, :], start=(kt == 0), stop=(kt == 7)), reads=[h32b, Wrb], writes=[pfb], skip_same=True)
                lg, lgb = lgr.next()
                P.op("dve", lambda e, lg=lg, pf=pf: e.tensor_tensor(out=lg[:, 0:36], in0=pf[:, 0:36], in1=brs[:], op=ALU.add), reads=[pfb, brb], writes=[lgb])
                sm, smb = smr.next()
                P.op("dve", lambda e, sm=sm, lg=lg: e.reduce_max(out=sm[:, 0:1], in_=lg[:, 0:4], axis=AX.X), reads=[lgb], writes=[smb])
                P.op("dve", lambda e, sm=sm: e.tensor_scalar(out=sm[:, 1:2], in0=sm[:, 0:1], scalar1=-1.0, scalar2=None, op0=ALU.mult), reads=[smb], writes=[smb])
                P.op("act", lambda e, sm=sm, lg=lg: e.activation(out=lg[:, 40:44], in_=lg[:, 0:4], func=AF.Exp, bias=sm[:, 1:2], accum_out=sm[:, 2:3]), reads=[lgb, smb], writes=[lgb, smb])
                P.op("dve", lambda e, sm=sm: e.reciprocal(out=sm[:, 3:4], in_=sm[:, 2:3]), reads=[smb], writes=[smb])
                P.op("dve", lambda e, sm=sm, lg=lg: e.tensor_scalar(out=sm[:, 4:8], in0=lg[:, 0:4], scalar1=sm[:, 0:1], scalar2=None, op0=ALU.is_ge), reads=[lgb, smb], writes=[smb])
                el, elb = elr.next()
                P.op("dve", lambda e, el=el, lg=lg, sm=sm: e.tensor_scalar(out=el[:], in0=lg[:, 4:12], scalar1=sm[:, 4:5], scalar2=None, op0=ALU.mult), reads=[lgb, smb], writes=[elb])
                for g in range(1, 4):
                    P.op("dve", lambda e, el=el, lg=lg, sm=sm, g=g: e.scalar_tensor_tensor(out=el[:], in0=lg[:, 4 + 8 * g:12 + 8 * g], scalar=sm[:, 4 + g:5 + g], in1=el[:], op0=ALU.mult, op1=ALU.add),
                         reads=[lgb, smb, elb], writes=[elb])
                m8, m8b = m8r.next()
                P.op("dve", lambda e, m8=m8, el=el: e.max(out=m8[:], in_=el[:]), reads=[elb], writes=[m8b])
                P.op("dve", lambda e, sm=sm, m8=m8: e.tensor_scalar(out=sm[:, 8:9], in0=m8[:, 0:1], scalar1=-1.0, scalar2=None, op0=ALU.mult), reads=[m8b, smb], writes=[smb])
                P.op("act", lambda e, lg=lg, el=el, sm=sm: e.activation(out=lg[:, 44:52], in_=el[:], func=AF.Exp, bias=sm[:, 8:9]), reads=[elb, smb], writes=[lgb])
                P.op("act", lambda e, sm=sm, m8=m8: e.activation(out=sm[:, 9:10], in_=m8[:, 1:2], func=AF.Exp, bias=sm[:, 8:9]), reads=[m8b, smb], writes=[smb])
                P.op("dve", lambda e, sm=sm: e.tensor_scalar_add(out=sm[:, 10:11], in0=sm[:, 9:10], scalar1=1.0), reads=[smb], writes=[smb])
                P.op("dve", lambda e, sm=sm: e.reciprocal(out=sm[:, 10:11], in_=sm[:, 10:11]), reads=[smb], writes=[smb])
                P.op("dve", lambda e, sm=sm: e.tensor_tensor(out=sm[:, 11:12], in0=sm[:, 10:11], in1=sm[:, 3:4], op=ALU.mult), reads=[smb], writes=[smb])
                P.op("dve", lambda e, lg=lg, el=el, m8=m8: e.tensor_scalar(out=lg[:, 52:60], in0=el[:], scalar1=m8[:, 1:2], scalar2=None, op0=ALU.is_ge), reads=[elb, m8b, lgb], writes=[lgb])
                P.op("dve", lambda e, lg=lg, sm=sm: e.scalar_tensor_tensor(out=lg[:, 44:52], in0=lg[:, 44:52], scalar=sm[:, 11:12], in1=lg[:, 52:60], op0=ALU.mult, op1=ALU.mult), reads=[lgb, smb], writes=[lgb])
                for g in range(4):
                    P.op("dve", lambda e, lg=lg, sm=sm, g=g, tt=tt: e.tensor_scalar(out=Wt[:, tt, 8 * g:8 * g + 8], in0=lg[:, 44:52], scalar1=sm[:, 4 + g:5 + g], scalar2=None, op0=ALU.mult),
                         reads=[lgb, smb], writes=[Wtb[tt]])
            P.barrier()
            P.emit()
        with ExitStack() as es:
            C = Ctx(nc, es, P)
            halves = [(0, (NG + 1) // 2), ((NG + 1) // 2, NG)]
            maxg = (NG + 1) // 2
            Y = C.sb([128, maxg * GT, D]); Yb = [Buf() for _ in range(maxg * GT)]
            H = C.sb([128, maxg * GT, 8, 128], BF16); Hb = Buf()
            for (g0, g1) in halves:
                nt_h = (g1 - g0) * GT
                if nt_h == 0:
                    continue
                for i in range(nt_h):
                    P.dma("sp", H[:, i, :, :], h2d[g0 * GT + i], writes=[Hb])
                    P.op("dve", lambda e, i=i: e.memset(Y[:, i, :], 0.0), writes=[Yb[i]])
                with ExitStack() as es2:
                    C2 = Ctx(nc, es2, P)
                    w1r = C2.ring(2, [128, 8, 512], BF16); w3r = C2.ring(2, [128, 8, 512], BF16); w2r = C2.ring(2, [128, 4, D], BF16)
                    hidr = C2.ring(2, [128, 4, GT * 128], BF16); sir = C2.ring(2, [128, GT * 128])
                    p1r = C2.ring(2, [128, 512], psum=True); p3r = C2.ring(2, [128, 512], psum=True); pyr = C2.ring(3, [128, 512], psum=True)
                    for ex in range(32):
                        a1, a1b = w1r.next(); a3, a3b = w3r.next(); a2, a2b = w2r.next()
                        v1 = w1[ex].rearrange("(kt p) c -> kt p c", p=128); v3 = w3[ex].rearrange("(kt p) c -> kt p c", p=128); v2 = w2[ex].rearrange("(kt p) c -> kt p c", p=128)
                        for kt in range(8):
                            P.dma("pool", a1[:, kt, :], v1[kt], writes=[a1b])
                            P.dma("pool", a3[:, kt, :], v3[kt], writes=[a3b])
                        for kt in range(4):
                            P.dma("pool", a2[:, kt, :], v2[kt], writes=[a2b])
                        for g in range(g1 - g0):
                            n = GT * 128
                            hid, hidb = hidr.next()
                            for j in range(4):
                                p1, p1b = p1r.next(); p3, p3b = p3r.next()
                                for kt in range(8):
                                    rhs = H[:, g * GT:(g + 1) * GT, kt, :]
                                    P.op("pe", lambda e, p1=p1, kt=kt, j=j, a1=a1, rhs=rhs, n=n: e.matmul(p1[:, 0:n], lhsT=a1[:, kt, j * 128:(j + 1) * 128], rhs=rhs, start=(kt == 0), stop=(kt == 7)),
                                         reads=[a1b, Hb], writes=[p1b], skip_same=True)
                                for kt in range(8):
                                    rhs = H[:, g * GT:(g + 1) * GT, kt, :]
                                    P.op("pe", lambda e, p3=p3, kt=kt, j=j, a3=a3, rhs=rhs, n=n: e.matmul(p3[:, 0:n], lhsT=a3[:, kt, j * 128:(j + 1) * 128], rhs=rhs, start=(kt == 0), stop=(kt == 7)),
                                         reads=[a3b, Hb], writes=[p3b], skip_same=True)
                                si, sib = sir.next()
                                P.op("act", lambda e, si=si, p1=p1, n=n: e.activation(out=si[:, 0:n], in_=p1[:, 0:n], func=AF.Silu), reads=[p1b], writes=[sib])
                                P.op("dve", lambda e, hid=hid, j=j, si=si, p3=p3, n=n: e.tensor_tensor(out=hid[:, j, 0:n], in0=p3[:, 0:n], in1=si[:, 0:n], op=ALU.mult), reads=[p3b, sib], writes=[hidb], skip_same=True)
                            for ti in range(GT):
                                i = g * GT + ti
                                tt = (g0 + g) * GT + ti
                                for ch in range(2):
                                    py, pyb = pyr.next()
                                    for j in range(4):
                                        P.op("pe", lambda e, py=py, hid=hid, j=j, ti=ti, ch=ch, a2=a2: e.matmul(py[:, 0:512], lhsT=hid[:, j, ti * 128:(ti + 1) * 128], rhs=a2[:, j, ch * 512:(ch + 1) * 512],
                                                                                                            start=(j == 0), stop=(j == 3)), reads=[hidb, a2b], writes=[pyb], skip_same=True)
                                    P.op("dve", lambda e, py=py, i=i, tt=tt, ch=ch, ex=ex: e.scalar_tensor_tensor(out=Y[:, i, ch * 512:(ch + 1) * 512], in0=py[:, 0:512], scalar=Wt[:, tt, ex:ex + 1],
                                                                                                                 in1=Y[:, i, ch * 512:(ch + 1) * 512], op0=ALU.mult, op1=ALU.add), reads=[pyb, Wtb[tt], Yb[i]], writes=[Yb[i]])
                    P.barrier()
                    P.emit()
                with ExitStack() as es2:
                    C2 = Ctx(nc, es2, P)
                    rows = C2.sb([128, 2, D]); rowb = Buf()
                    for i in range(2):
                        P.dma("sp", rows[:, i, :], lnrows[2 + i], writes=[rowb])
                    x1r = C2.ring(2, [128, D]); tr_ = C2.ring(2, [128, D]); tmpr = C2.ring(2, [128, 512]); outr = C2.ring(2, [128, D])
                    rings = {"st": C2.ring(2, [128, 8]), "sq": C2.ring(1, [128, D])}
                    for i in range(nt_h):
                        tt = g0 * GT + i
                        s = 0 if tt < 2 else 1
                        x1, x1b = x1r.next()
                        P.dma("sp", x1[:], x1d[tt * 128:(tt + 1) * 128, :], writes=[x1b])
                        t, tb = tr_.next()
                        for ch in range(2):
                            tmp, tmpb = tmpr.next()
                            P.op("pool", lambda e, tmp=tmp, i=i, ch=ch, s=s: e.tensor_tensor(out=tmp[:], in0=Y[:, i, ch * 512:(ch + 1) * 512], in1=g12[:, 1, s, ch * 512:(ch + 1) * 512], op=ALU.mult), reads=[Yb[i], g12b], writes=[tmpb])
                            P.op("dve", lambda e, t=t, x1=x1, tmp=tmp, ch=ch: e.scalar_tensor_tensor(out=t[:, ch * 512:(ch + 1) * 512], in0=x1[:, ch * 512:(ch + 1) * 512], scalar=ALPHA, in1=tmp[:], op0=ALU.mult, op1=ALU.add),
                                 reads=[x1b, tmpb], writes=[tb])
                        o, ob_ = outr.next()
                        _layer_norm(P, C2, rings, t, tb, rows[:, 0, :], rowb, rows[:, 1, :], rowb, o, ob_)
                        P.dma("sp", xout[tt * 128:(tt + 1) * 128, :], o[:], reads=[ob_])
                    P.barrier()
                    P.emit()
    return nc


def b_inputs(x, y3, cA, cB, w_mod, b_mod, w_in, wa, wb, wc, wo, l1g, l1b, l2g, l2b, wgrp, bgrp, wrt, brt, w1, w3, w2):
    cst = consts_np()
    bc = lambda v: np.broadcast_to(np.asarray(v, np.float32)[None], (128, v.shape[0]))
    return {
        "xin": np.ascontiguousarray(x, np.float32), "y3": np.ascontiguousarray(y3, np.float32),
        "c2": np.stack([cA.reshape(8, 128).T, cB.reshape(8, 128).T], -1).astype(np.float32).copy(),
        "wmod": np.ascontiguousarray(w_mod), "bmod": np.ascontiguousarray(b_mod.reshape(48, 128).T),
        "wg": np.ascontiguousarray(w_in[:, O_GATES:]), "wbr": np.stack([wa, wb, wc]), "wo": np.ascontiguousarray(wo),
        "lnrows": np.stack([bc(l1g), bc(l1b), bc(l2g), bc(l2b)]).astype(np.float32).copy(),
        "wr": np.concatenate([wgrp, wrt], 1).astype(np.float32).copy(), "brr": bc(np.concatenate([bgrp, brt])).astype(np.float32).copy(),
        "w1": w1, "w3": w3, "w2": w2, "k_ident": cst["ident"], "k_ones": cst["ones"],
    }


def hgrn_params(hh, layer, lb_logits, nw):
    sub = lb_logits[:, :, hh * 256:(hh + 1) * 256]
    lblc = sub.reshape(2, DEPTH, 2, 128).transpose(3, 1, 0, 2).reshape(128, DEPTH, 4)
    lblr = np.broadcast_to(sub.transpose(1, 0, 2).reshape(1, DEPTH, 512), (128, DEPTH, 512))
    m = np.array([1.0 if 1 <= i <= layer else 0.0 for i in range(DEPTH)], np.float32)
    return {"lblc": np.ascontiguousarray(lblc, np.float32), "lblr": np.ascontiguousarray(lblr, np.float32),
            "lmask": np.broadcast_to(m[None], (128, DEPTH)).copy(), "cnw": np.broadcast_to(nw[None], (128, 128)).astype(np.float32).copy()}


_PROGS = {}


def _prog(key, fn):
    if key not in _PROGS:
        _PROGS[key] = fn()
    return _PROGS[key]


def kernel(x, c, ctx, c_ctx, w_mod, b_mod, w_in, conv_a, gdn_a_log, gdn_dt_bias, gdn_norm_w,
           attn_sink, hgrn_lb_logits, hgrn_norm_w, w_branch_a, w_branch_b, w_branch_c, w_out,
           ln1_g, ln1_b, ln2_g, ln2_b, w_group, b_group, w_router, b_router, w1, w3, w2):
    f32 = lambda a: np.asarray(a, np.float32)
    x, c, ctx, c_ctx = f32(x), f32(c), f32(ctx), f32(c_ctx)
    bsz, nlat, _ = x.shape
    T = NCTX + nlat
    NT = T // 128
    X = [np.concatenate([ctx[b], x[b]], 0) for b in range(bsz)]
    cst = {"k_" + k: v for k, v in consts_np().items()}
    ropeC, ropeS = rope_tables(nlat)
    ncA = _prog(("A", nlat), lambda: build_A(nlat))
    half = NT // 2
    ncB = _prog(("B", half), lambda: build_B(half * 128))
    col = lambda v: np.ascontiguousarray(f32(v).reshape(8, 128).T)
    for l in range(DEPTH):
        wm, bm, wi = f32(w_mod[l]), f32(b_mod[l]), f32(w_in[l])
        in_maps = []
        for b in range(bsz):
            for hh in range(2):
                m = {"xin": X[b], "c2": np.stack([col(c[b]), col(c_ctx)], -1).copy(),
                     "wmod": np.ascontiguousarray(wm[:, :2048]), "bmod": np.ascontiguousarray(bm[:2048].reshape(16, 128).T),
                     "wfm": np.ascontiguousarray(wi[:, fm_columns(hh)]), "wtm": np.ascontiguousarray(wi[:, tm_columns(hh)]),
                     "ropeC": ropeC, "ropeS": ropeS,
                     "sink": np.broadcast_to(f32(attn_sink[l])[hh * 4:hh * 4 + 4][None], (128, 4)).copy()}
                m.update(cst)
                m.update(hgrn_params(hh, l, f32(hgrn_lb_logits), f32(hgrn_norm_w[l])))
                m.update(gdn_params(hh, f32(conv_a[l]), f32(gdn_a_log[l]), f32(gdn_dt_bias[l]), f32(gdn_norm_w[l])))
                in_maps.append(m)
        res = run_bass_kernel_spmd(ncA, in_maps, core_ids=list(range(8))).results
        y3 = []
        for b in range(bsz):
            parts = []
            for nm in ("ya", "yb", "yc"):
                parts += [res[2 * b][nm], res[2 * b + 1][nm]]
            y3.append(np.concatenate(parts, 1))
        in_maps = []
        for b in range(bsz):
            for j in range(2):
                sl = slice(j * half * 128, (j + 1) * half * 128)
                cA = c_ctx if j == 0 else c[b]
                in_maps.append(b_inputs(X[b][sl], y3[b][sl], cA, c[b], wm, bm, wi, f32(w_branch_a[l]), f32(w_branch_b[l]), f32(w_branch_c[l]), f32(w_out[l]),
                                        f32(ln1_g[l]), f32(ln1_b[l]), f32(ln2_g[l]), f32(ln2_b[l]), f32(w_group[l]), f32(b_group[l]), f32(w_router[l]), f32(b_router[l]),
                                        f32(w1[l]), f32(w3[l]), f32(w2[l])))
        res = run_bass_kernel_spmd(ncB, in_maps, core_ids=list(range(8))).results
        X = [np.concatenate([res[2 * b]["xout"], res[2 * b + 1]["xout"]], 0) for b in range(bsz)]
    return np.stack([X[b][NCTX:] for b in range(bsz)], 0).astype(np.float32)
```
